# Optimizing a Trainium2 kernel written in Bass

```python
import jax, jax.numpy as jnp
from jax import lax
import numpy as np

D_MODEL = 1024
BATCH = 2
SEQ = 8192
DEPTH = 1

CHUNK = 64
N_MEM = 256
A_HEADS = 8
A_HEAD_DIM = 64
KV_RANK = 128
IDX_HEADS = 8
IDX_DIM = 32
TOPK_MAX = 256
Q_BLOCK = 128
CONV_CH = 512
CONV_WIDTH = 31
C_HEADS = 4
C_HEAD_DIM = 128
N_BRANCH = 3
N_EXPERTS = 32
TOP_K = 4
D_EXPERT = 1024
SWIGLU_LIMIT = 7.0
SWIGLU_ALPHA = 1.702
MOE_BLOCK = 256

LN_EPS = 1e-5
NEG_INF = -1e30
A_SCALE = A_HEAD_DIM ** -0.5
IDX_SCALE = (IDX_HEADS * IDX_DIM) ** -0.5
C_SCALE = C_HEAD_DIM ** -0.5
DN_ALPHA = (2 * DEPTH) ** 0.25
DN_BETA = (8 * DEPTH) ** -0.25

SPLITS = (A_HEADS * A_HEAD_DIM, KV_RANK, IDX_HEADS * IDX_DIM, IDX_DIM, IDX_HEADS,
          2 * CONV_CH, C_HEADS * C_HEAD_DIM, N_BRANCH * D_MODEL)
D_IN = sum(SPLITS)

kernel_name = 'hybrid_dsa_conformer_memory_moe_deepnorm'


def _layer_norm(x, g, b):
    xf = x.astype(jnp.float32)
    xc = xf - jnp.mean(xf, -1, keepdims=True)
    var = jnp.mean(xc * xc, -1, keepdims=True)
    return (xc * lax.rsqrt(var + LN_EPS) * g.astype(jnp.float32) + b.astype(jnp.float32)).astype(x.dtype)


def _rms_norm(x, g):
    xf = x.astype(jnp.float32)
    return (xf * lax.rsqrt(jnp.mean(xf * xf, -1, keepdims=True) + LN_EPS) * g.astype(jnp.float32)).astype(x.dtype)


def _split_cols(a):
    parts, start = [], 0
    for width in SPLITS:
        parts.append(a[..., start:start + width])
        start += width
    return parts


def _alibi_slopes():
    return jnp.exp2(-(8.0 / A_HEADS) * jnp.arange(1, A_HEADS + 1, dtype=jnp.float32))


def _dsa_mla_attention(q_a, c_kv, q_idx, k_idx, w_idx, w_uk, w_uv):
    bsz, seq = c_kv.shape[:2]
    topk = min(TOPK_MAX, seq // 4)
    n_blocks = seq // Q_BLOCK
    q_lat = jnp.einsum('bthd,hrd->bthr', q_a, w_uk)
    key_chunk = jnp.arange(seq) // CHUNK
    slopes = _alibi_slopes()
    k_idx32 = k_idx.astype(jnp.float32)
    c_kv32 = c_kv.astype(jnp.float32)

    def to_blocks(a):
        return jnp.moveaxis(a.reshape(bsz, n_blocks, Q_BLOCK, *a.shape[2:]), 1, 0)

    def block(args):
        qi, wi, ql, blk = args
        q_pos = blk * Q_BLOCK + jnp.arange(Q_BLOCK)
        q_chunk = q_pos // CHUNK
        admissible = key_chunk[None, :] <= q_chunk[:, None]
        rel = jax.nn.relu(jnp.einsum('bqhd,bsd->bqhs', qi.astype(jnp.float32), k_idx32))
        score = jnp.einsum('bqhs,bqh->bqs', rel, wi.astype(jnp.float32) * IDX_SCALE)
        score = jnp.where(admissible[None], score, NEG_INF)
        _, sel = lax.top_k(score, topk)
        c_sel = jax.vmap(lambda c, i: c[i])(c_kv32, sel)
        valid = key_chunk[sel] <= q_chunk[None, :, None]
        dist = jnp.abs(q_pos[None, :, None] - sel).astype(jnp.float32)
        logits = (jnp.einsum('bqhr,bqkr->bqhk', ql.astype(jnp.float32), c_sel) * A_SCALE
                  - slopes[None, None, :, None] * dist[:, :, None, :])
        logits = jnp.where(valid[:, :, None, :], logits, NEG_INF)
        p = jax.nn.softmax(logits, axis=-1)
        return jnp.einsum('bqhk,bqkr->bqhr', p, c_sel).astype(ql.dtype)

    o_lat = lax.map(block, (to_blocks(q_idx), to_blocks(w_idx), to_blocks(q_lat), jnp.arange(n_blocks)))
    o_lat = jnp.moveaxis(o_lat, 0, 1).reshape(bsz, seq, A_HEADS, KV_RANK)
    return jnp.einsum('bthr,hrd->bthd', o_lat, w_uv).reshape(bsz, seq, A_HEADS * A_HEAD_DIM)


def _conformer_conv(glu_in, w_dw, b_dw, ln_g, ln_b, w_pw2, b_pw2):
    a, g = jnp.split(glu_in, 2, axis=-1)
    u = a * jax.nn.sigmoid(g)
    u = lax.conv_general_dilated(u, w_dw[:, None, :], window_strides=(1,),
                                 padding=[(CONV_WIDTH - 1, 0)],
                                 dimension_numbers=('NWC', 'WIO', 'NWC'),
                                 feature_group_count=CONV_CH) + b_dw
    u = jax.nn.silu(_layer_norm(u, ln_g, ln_b))
    return u @ w_pw2 + b_pw2


def _memory_attention(q_c, mem, w_mem_k, w_mem_v):
    bsz, seq = q_c.shape[:2]
    n_mem = mem.shape[1]
    q = q_c.reshape(bsz, seq, C_HEADS, C_HEAD_DIM).astype(jnp.float32)
    k = (mem @ w_mem_k).reshape(bsz, n_mem, C_HEADS, C_HEAD_DIM).astype(jnp.float32)
    v = (mem @ w_mem_v).reshape(bsz, n_mem, C_HEADS, C_HEAD_DIM).astype(jnp.float32)
    p = jax.nn.softmax(jnp.einsum('bthd,bmhd->bhtm', q, k) * C_SCALE, axis=-1)
    o = jnp.einsum('bhtm,bmhd->bthd', p, v)
    return o.reshape(bsz, seq, C_HEADS * C_HEAD_DIM).astype(q_c.dtype)


def _moe(x, w_router, b_router, w1, b1, w2, b2):
    bsz, seq, dm = x.shape
    n_tok = bsz * seq
    xt = x.reshape(n_tok, dm)
    logits = (xt @ w_router + b_router).astype(jnp.float32)
    top_v, top_e = lax.top_k(logits, TOP_K)
    gate = jax.nn.softmax(top_v, axis=-1)
    n_assign = n_tok * TOP_K
    flat_e = top_e.reshape(n_assign)
    flat_tok = jnp.arange(n_assign) // TOP_K
    flat_g = gate.reshape(n_assign)
    order = jnp.argsort(flat_e)
    sorted_e = flat_e[order]
    counts = jnp.bincount(flat_e, length=N_EXPERTS)
    padded = (counts + MOE_BLOCK - 1) // MOE_BLOCK * MOE_BLOCK
    pad_end = jnp.cumsum(padded)
    pad_start = pad_end - padded
    grp_start = jnp.cumsum(counts) - counts
    dest = pad_start[sorted_e] + jnp.arange(n_assign) - grp_start[sorted_e]
    n_blocks = -(-n_assign // MOE_BLOCK) + N_EXPERTS
    n_rows = n_blocks * MOE_BLOCK
    row_tok = jnp.full((n_rows,), n_tok, dtype=jnp.int32).at[dest].set(flat_tok[order])
    row_gate = jnp.zeros((n_rows,), jnp.float32).at[dest].set(flat_g[order])
    blk_exp = jnp.minimum(jnp.searchsorted(pad_end, jnp.arange(n_blocks) * MOE_BLOCK, side='right'),
                          N_EXPERTS - 1)
    x_pad = jnp.concatenate([xt, jnp.zeros((1, dm), xt.dtype)], axis=0)
    rows = x_pad[row_tok].reshape(n_blocks, MOE_BLOCK, dm)

    def expert_block(args):
        xb, e = args
        hdn = xb @ w1[e] + b1[e]
        g, u = jnp.split(hdn, 2, axis=-1)
        g = jnp.minimum(g, SWIGLU_LIMIT)
        u = jnp.clip(u, -SWIGLU_LIMIT, SWIGLU_LIMIT)
        return ((u + 1.0) * (g * jax.nn.sigmoid(SWIGLU_ALPHA * g))) @ w2[e] + b2[e]

    y = lax.map(expert_block, (rows, blk_exp)).reshape(n_rows, dm)
    y = y * row_gate[:, None].astype(y.dtype)
    out = jax.ops.segment_sum(y, row_tok, num_segments=n_tok + 1)[:n_tok]
    return out.reshape(bsz, seq, dm)


def setup_inputs(seed: int = 0) -> dict:
    key = jax.random.key(seed)
    ks = jax.random.split(key, 32)
    L = DEPTH
    a_w = A_HEADS * A_HEAD_DIM
    c_w = C_HEADS * C_HEAD_DIM

    def nrm(k, shape, scale):
        return scale * jax.random.normal(k, shape, jnp.float32)

    return {
        'x': nrm(ks[0], (BATCH, SEQ, D_MODEL), 1.0),
        'mem': nrm(ks[1], (BATCH, N_MEM, D_MODEL), 1.0),
        'ln0_g': 1.0 + nrm(ks[2], (D_MODEL,), 0.02),
        'ln0_b': nrm(ks[3], (D_MODEL,), 0.02),
        'w_in': nrm(ks[4], (L, D_MODEL, D_IN), D_MODEL ** -0.5),
        'b_in': nrm(ks[5], (L, D_IN), 0.02),
        'kv_norm_g': 1.0 + nrm(ks[6], (L, KV_RANK), 0.02),
        'w_uk': nrm(ks[7], (L, A_HEADS, KV_RANK, A_HEAD_DIM), KV_RANK ** -0.5),
        'w_uv': nrm(ks[8], (L, A_HEADS, KV_RANK, A_HEAD_DIM), DN_BETA * KV_RANK ** -0.5),
        'w_o_a': nrm(ks[9], (L, a_w, D_MODEL), DN_BETA * a_w ** -0.5),
        'w_dw': nrm(ks[10], (L, CONV_WIDTH, CONV_CH), CONV_WIDTH ** -0.5),
        'b_dw': nrm(ks[11], (L, CONV_CH), 0.02),
        'conv_ln_g': 1.0 + nrm(ks[12], (L, CONV_CH), 0.02),
        'conv_ln_b': nrm(ks[13], (L, CONV_CH), 0.02),
        'w_pw2': nrm(ks[14], (L, CONV_CH, D_MODEL), DN_BETA * CONV_CH ** -0.5),
        'b_pw2': nrm(ks[15], (L, D_MODEL), 0.02),
        'w_mem_k': nrm(ks[16], (L, D_MODEL, c_w), D_MODEL ** -0.5),
        'w_mem_v': nrm(ks[17], (L, D_MODEL, c_w), DN_BETA * D_MODEL ** -0.5),
        'w_o_c': nrm(ks[18], (L, c_w, D_MODEL), DN_BETA * c_w ** -0.5),
        'w_out': nrm(ks[19], (L, D_MODEL, D_MODEL), DN_BETA * D_MODEL ** -0.5),
        'b_out': nrm(ks[20], (L, D_MODEL), 0.02),
        'ln1_g': 1.0 + nrm(ks[21], (L, D_MODEL), 0.02),
        'ln1_b': nrm(ks[22], (L, D_MODEL), 0.02),
        'w_router': nrm(ks[23], (L, D_MODEL, N_EXPERTS), D_MODEL ** -0.5),
        'b_router': nrm(ks[24], (L, N_EXPERTS), 0.01),
        'w1': nrm(ks[25], (L, N_EXPERTS, D_MODEL, 2 * D_EXPERT), DN_BETA * D_MODEL ** -0.5),
        'b1': nrm(ks[26], (L, N_EXPERTS, 2 * D_EXPERT), 0.01),
        'w2': nrm(ks[27], (L, N_EXPERTS, D_EXPERT, D_MODEL), DN_BETA * D_EXPERT ** -0.5),
        'b2': nrm(ks[28], (L, N_EXPERTS, D_MODEL), 0.01),
        'ln2_g': 1.0 + nrm(ks[29], (L, D_MODEL), 0.02),
        'ln2_b': nrm(ks[30], (L, D_MODEL), 0.02),
    }


def reference(x, mem, ln0_g, ln0_b, w_in, b_in, kv_norm_g, w_uk, w_uv, w_o_a,
              w_dw, b_dw, conv_ln_g, conv_ln_b, w_pw2, b_pw2,
              w_mem_k, w_mem_v, w_o_c, w_out, b_out, ln1_g, ln1_b,
              w_router, b_router, w1, b1, w2, b2, ln2_g, ln2_b):
    bsz, seq, dm = x.shape
    h = _layer_norm(x, ln0_g, ln0_b)
    for l in range(DEPTH):
        proj = h @ w_in[l] + b_in[l]
        q_a, c_kv, q_idx, k_idx, w_idx, glu_in, q_c, gate_logits = _split_cols(proj)
        c_kv = _rms_norm(c_kv, kv_norm_g[l])
        y_a = _dsa_mla_attention(q_a.reshape(bsz, seq, A_HEADS, A_HEAD_DIM), c_kv,
                                 q_idx.reshape(bsz, seq, IDX_HEADS, IDX_DIM), k_idx, w_idx,
                                 w_uk[l], w_uv[l]) @ w_o_a[l]
        y_b = _conformer_conv(glu_in, w_dw[l], b_dw[l], conv_ln_g[l], conv_ln_b[l], w_pw2[l], b_pw2[l])
        y_c = _memory_attention(q_c, mem, w_mem_k[l], w_mem_v[l]) @ w_o_c[l]
        g = jax.nn.sigmoid(gate_logits).reshape(bsz, seq, N_BRANCH, dm)
        mixed = g[:, :, 0] * y_a + g[:, :, 1] * y_b + g[:, :, 2] * y_c
        h = _layer_norm(DN_ALPHA * h + (mixed @ w_out[l] + b_out[l]), ln1_g[l], ln1_b[l])
        h = _layer_norm(DN_ALPHA * h + _moe(h, w_router[l], b_router[l], w1[l], b1[l], w2[l], b2[l]),
                        ln2_g[l], ln2_b[l])
    return h
```

```python
import numpy as np
import ml_dtypes
import concourse.bass as bass
import concourse.mybir as mybir
from concourse.bass_utils import run_bass_kernel_spmd
from contextlib import ExitStack

F32 = mybir.dt.float32
BF16 = mybir.dt.bfloat16
FP8 = mybir.dt.float8e5
ALU = mybir.AluOpType
AF = mybir.ActivationFunctionType
AX = mybir.AxisListType

D = 1024
SEQ = 8192
TQ = 2048
NT = 16
NKT = 64
D_IN = 5544
OFF_QA, OFF_CKV, OFF_QIDX, OFF_KIDX, OFF_WIDX, OFF_GLU, OFF_QC, OFF_GATE = 0, 512, 640, 896, 928, 936, 1960, 2472
LN_EPS = 1e-5
IDX_SCALE = 256 ** -0.5
DN_ALPHA = 2 ** 0.25
NEG = -1e30
MB = -28672.0
NITER = 14

_ES = {F32: 4, BF16: 2, FP8: 1, mybir.dt.int32: 4, mybir.dt.int16: 2, mybir.dt.uint16: 2, mybir.dt.uint32: 4}


def _esize(dt):
    return _ES.get(dt, 4)


class Sched:
    def __init__(self, nc, ndma=4):
        self.nc = nc
        self.e = dict(pe=nc.tensor, act=nc.scalar, dve=nc.vector, pool=nc.gpsimd, sp=nc.sync)
        self.sem = {k: nc.alloc_semaphore(name="s_" + k) for k in ("pe", "act", "dve", "pool")}
        self.cnt = {k: 0 for k in self.sem}
        self.ndma = ndma
        self.dsem = {}
        self.dcnt = {}
        for q in ("sp", "pool", "act"):
            self.dsem[q] = [nc.alloc_semaphore(name="d_%s%d" % (q, i)) for i in range(ndma)]
            self.dcnt[q] = 0
        self.seen = {k: {} for k in self.e}
        self.regs = {}
        self.nwait = 0
        self.nins = 0

    def region(self, ap):
        t = ap.tensor
        tn = type(t).__name__
        es = _esize(ap.dtype)
        dims = list(ap.ap)
        if "DRam" in tn:
            ext = sum((c - 1) * abs(s) for s, c in dims) + 1
            return ("D:" + t.name, 0, 1, ap.offset * es, (ap.offset + ext) * es)
        shape = list(t.shape)
        fsz = 1
        for s in shape[1:]:
            fsz *= s
        off = ap.offset
        p0 = off // fsz
        f0 = off % fsz
        pstep, pcnt = dims[0]
        if pstep == 0:
            pcnt = 1
        ext = sum((c - 1) * abs(s) for s, c in dims[1:]) + 1
        if "PSum" in tn:
            b0 = (f0 * es) // 2048 * 2048
            b1 = ((f0 + ext) * es + 2047) // 2048 * 2048
            return (t.name, 0, 128, b0, b1)
        return (t.name, p0, p0 + pcnt, f0 * es, (f0 + ext) * es)

    @staticmethod
    def _ov(a, b):
        return a[1] < b[2] and b[1] < a[2] and a[3] < b[4] and b[3] < a[4]

    def _deps_read(self, reg, deps):
        for rec in self.regs.get(reg[0], ()):
            if self._ov(rec[0], reg):
                for k, v in rec[1].items():
                    if deps.get(k, 0) < v:
                        deps[k] = v

    def _deps_write(self, reg, deps):
        for rec in self.regs.get(reg[0], ()):
            if self._ov(rec[0], reg):
                for dd in (rec[1], rec[2]):
                    for k, v in dd.items():
                        if deps.get(k, 0) < v:
                            deps[k] = v

    def _mark_read(self, reg, ev):
        k, v = ev
        for rec in self.regs.get(reg[0], ()):
            if self._ov(rec[0], reg):
                if rec[2].get(k, 0) < v:
                    rec[2][k] = v

    def _mark_write(self, reg, ev):
        lst = self.regs.setdefault(reg[0], [])
        keep = []
        for rec in lst:
            r = rec[0]
            if reg[1] <= r[1] and r[2] <= reg[2] and reg[3] <= r[3] and r[4] <= reg[4]:
                continue
            keep.append(rec)
        keep.append([reg, {ev[0]: ev[1]}, {}])
        if len(keep) > 40:
            p0 = min(r[0][1] for r in keep)
            p1 = max(r[0][2] for r in keep)
            f0 = min(r[0][3] for r in keep)
            f1 = max(r[0][4] for r in keep)
            w, rd = {}, {}
            for r in keep:
                for k, v in r[1].items():
                    w[k] = max(w.get(k, 0), v)
                for k, v in r[2].items():
                    rd[k] = max(rd.get(k, 0), v)
            keep = [[(reg[0], p0, p1, f0, f1), w, rd]]
        self.regs[reg[0]] = keep

    def _semof(self, key):
        if isinstance(key, tuple):
            return self.dsem[key[1]][key[2]]
        return self.sem[key]

    def _emit_waits(self, eng, deps, skip_same=None):
        for k, v in deps.items():
            if self.seen[eng].get(k, 0) >= v:
                continue
            self.e[eng].wait_ge(self._semof(k), v)
            self.seen[eng][k] = v
            self.nwait += 1

    def op(self, eng, fn, reads=(), writes=()):
        deps = {}
        rregs = [self.region(a) for a in reads if a is not None and hasattr(a, "tensor")]
        wregs = [self.region(a) for a in writes if a is not None and hasattr(a, "tensor")]
        for r in rregs:
            self._deps_read(r, deps)
        wdeps = {}
        for w in wregs:
            self._deps_write(w, wdeps)
        for k, v in wdeps.items():
            if k == eng and eng == "pe":
                continue
            if deps.get(k, 0) < v:
                deps[k] = v
        self._emit_waits(eng, deps)
        ins = fn()
        self.cnt[eng] += 1
        ev = (eng, self.cnt[eng])
        ins.then_inc(self.sem[eng], 1)
        self.nins += 1
        for r in rregs:
            self._mark_read(r, ev)
        for w in wregs:
            self._mark_write(w, ev)
        return ins

    def dma(self, q, out, in_, **kw):
        deps = {}
        rr = self.region(in_)
        wr = self.region(out)
        track_r = not rr[0].startswith("D:") or rr[0] in self.regs or rr[0].startswith("D:scr")
        track_w = True
        if track_r:
            self._deps_read(rr, deps)
        self._deps_write(wr, deps)
        n = self.dcnt[q]
        slot = n % self.ndma
        key = ("dma", q, slot)
        prev = 16 * (n // self.ndma)
        if prev > 0:
            deps[key] = max(deps.get(key, 0), prev)
        self._emit_waits(q, deps)
        ins = self.e[q].dma_start(out=out, in_=in_, **kw)
        val = 16 * (n // self.ndma + 1)
        ins.then_inc(self.dsem[q][slot], 16)
        self.dcnt[q] = n + 1
        ev = (key, val)
        if track_r:
            self._mark_read(rr, ev)
        if track_w:
            self._mark_write(wr, ev)
        self.nins += 1
        return ins

    def barrier(self):
        deps = {}
        for q in ("sp", "pool", "act"):
            n = self.dcnt[q]
            for slot in range(self.ndma):
                cntslot = (n - slot + self.ndma - 1) // self.ndma if n > slot else 0
                if cntslot > 0:
                    deps[("dma", q, slot)] = 16 * cntslot
        for k in self.sem:
            if self.cnt[k] > 0:
                deps[k] = self.cnt[k]
        for eng in self.e:
            self._emit_waits(eng, dict(deps))

    def finish(self):
        deps = {}
        for q in ("sp", "pool", "act"):
            n = self.dcnt[q]
            for slot in range(self.ndma):
                cntslot = (n - slot + self.ndma - 1) // self.ndma if n > slot else 0
                if cntslot > 0:
                    deps[("dma", q, slot)] = 16 * cntslot
        for k in self.sem:
            if self.cnt[k] > 0:
                deps[k] = self.cnt[k]
        self._emit_waits("sp", deps)

    def mm(self, out, lhsT, rhs, start=True, stop=True):
        return self.op("pe", lambda: self.nc.tensor.matmul(out, lhsT, rhs, start=start, stop=stop,
                                                          skip_group_check=True),
                       reads=(lhsT, rhs), writes=(out,))

    def tr(self, out, in_, ident):
        return self.op("pe", lambda: self.nc.tensor.transpose(out, in_, ident), reads=(in_, ident), writes=(out,))

    def act(self, out, in_, func, bias=None, scale=None, accum_out=None):
        kw = {}
        if bias is not None:
            kw["bias"] = bias
        if scale is not None:
            kw["scale"] = scale
        if accum_out is not None:
            kw["accum_out"] = accum_out
        return self.op("act", lambda: self.nc.scalar.activation(out, in_, func, **kw),
                       reads=(in_, bias, scale), writes=(out, accum_out))

    def ts(self, eng, out, in0, s1, s2, op0, op1=None, accum_out=None):
        e = self.e[eng]
        kw = {}
        if op1 is not None:
            kw["op1"] = op1
        if accum_out is not None:
            kw["accum_out"] = accum_out
        if out.dtype == FP8:
            kw["saturate"] = False
        return self.op(eng, lambda: e.tensor_scalar(out, in0, s1, s2, op0, **kw),
                       reads=(in0, s1, s2), writes=(out, accum_out))

    def tt(self, eng, out, in0, in1, op):
        e = self.e[eng]
        return self.op(eng, lambda: e.tensor_tensor(out, in0, in1, op), reads=(in0, in1), writes=(out,))

    def stt(self, eng, out, in0, scalar, in1, op0, op1):
        e = self.e[eng]
        return self.op(eng, lambda: e.scalar_tensor_tensor(out, in0, scalar, in1, op0, op1),
                       reads=(in0, scalar, in1), writes=(out,))

    def cp(self, eng, out, in_):
        e = self.e[eng]
        if eng == "act":
            return self.op(eng, lambda: e.copy(out, in_), reads=(in_,), writes=(out,))
        return self.op(eng, lambda: e.tensor_copy(out, in_), reads=(in_,), writes=(out,))

    def memset(self, eng, out, val):
        e = self.e[eng]
        return self.op(eng, lambda: e.memset(out, val), writes=(out,))

    def recip(self, out, in_):
        return self.op("dve", lambda: self.nc.vector.reciprocal(out, in_), reads=(in_,), writes=(out,))

    def red(self, out, in_, op, axis=AX.X):
        return self.op("dve", lambda: self.nc.vector.tensor_reduce(out, in_, axis, op), reads=(in_,), writes=(out,))

    def max8(self, out, in_):
        return self.op("dve", lambda: self.nc.vector.max(out, in_), reads=(in_,), writes=(out,))


def _consts():
    slopes = np.exp2(-(np.arange(1, 9, dtype=np.float64)))
    ident = np.eye(128, dtype=np.float32)
    E = np.tile(ident, (1, 8)).astype(np.float32)
    sl = np.arange(128)[:, None]
    tl = np.arange(128)[None, :]
    cd = np.zeros((128, 8, 128), np.float32)
    for h in range(8):
        cd[:, h, :] = 8.0 * slopes[h] * (tl - np.abs(tl - sl))
    cd = cd.reshape(128, 1024)
    spos = np.arange(SEQ)
    lpos = np.zeros((48, SEQ), np.float32)
    lpos[0:16] = 1.0
    lpos[32:40] = (spos & 127)[None, :]
    lpos[40:48] = (spos >> 7)[None, :]
    ldiag = np.zeros((48, NT, 128), np.float32)
    ldiag[0:16] = 1.0
    for i in range(NT):
        ldiag[40:48, i, :] = 48 + i
    rb = np.zeros((48, 8, 128), np.float32)
    bd = np.zeros((16, 8, 128), np.float32)
    for h in range(8):
        rb[32 + h, h, :] = 8.0 * slopes[h]
        rb[40 + h, h, :] = 8.0 * 128.0 * slopes[h]
        bd[h, h, :] = 1.0
        bd[8 + h, h, :] = 1.0
    rb = rb.reshape(48, 1024)
    bd = bd.reshape(16, 1024)
    dg = np.zeros((128, 128), np.float32)
    dg[:64, 64:] = NEG
    slope8 = np.tile((8.0 * slopes).astype(np.float32)[None, :], (128, 1))
    jpos = np.tile(((np.arange(128) + 1) * 64).astype(np.float32)[None, :], (128, 1))
    tlc = np.arange(128, dtype=np.float32)[:, None].copy()
    half = np.tile((0.5 ** (np.arange(NITER) + 1)).astype(np.float32)[None, :], (128, 1))
    return dict(ident=ident, E=E, cd=cd, lpos=lpos, ldiag=ldiag.reshape(48, NT * 128), rb=rb, bd=bd, dg=dg,
                slope8=slope8, jpos=jpos, tl=tlc, half=half)


def build(stage=99, dbg=(), lim=None):
    lim = lim or {}
    nc = bass.Bass("TRN2", target_bir_lowering=False)
    S = Sched(nc)
    dbg_out = {}

    def din(name, shape, dt=F32):
        return nc.dram_tensor(name, list(shape), dt, kind="ExternalInput").ap()

    xk = din("xk", [SEQ, D])
    cvec = din("cvec", [128, 8])
    ln0_g = din("ln0_g", [1, D]); ln0_b = din("ln0_b", [1, D])
    w_in = din("w_in", [D, D_IN]); b_in = din("b_in", [1, D_IN])
    kv_norm_g = din("kv_norm_g", [1, 128])
    C = {k: din("c_" + k, v.shape) for k, v in _consts().items()}
    if stage >= 2:
        w_uk = din("w_uk", [8, 128, 64]); w_uv = din("w_uv", [8, 128, 64])
        w_o_a = din("w_o_a", [512, D])
    if stage >= 3:
        memb = din("memb", [256, D])
        w_dw = din("w_dw", [31, 512]); b_dw = din("b_dw", [1, 512])
        conv_ln_g = din("conv_ln_g", [1, 512]); conv_ln_b = din("conv_ln_b", [1, 512])
        w_pw2 = din("w_pw2", [512, D]); b_pw2 = din("b_pw2", [1, D])
        w_mem_k = din("w_mem_k", [D, 512]); w_mem_v = din("w_mem_v", [D, 512])
        w_o_c = din("w_o_c", [512, D])
        w_out = din("w_out", [D, D]); b_out = din("b_out", [1, D])
        ln1_g = din("ln1_g", [1, D]); ln1_b = din("ln1_b", [1, D])
    if stage >= 4:
        w_router = din("w_router", [D, 32]); b_router = din("b_router", [1, 32])
        w1 = din("w1", [32, D, 2048]); b1 = din("b1", [32, 2048])
        w2 = din("w2", [32, D, D]); b2 = din("b2", [32, D])
        ln2_g = din("ln2_g", [1, D]); ln2_b = din("ln2_b", [1, D])
    y_out = nc.dram_tensor("y", [TQ, D], F32, kind="ExternalOutput").ap()

    def dbg_tensor(name, shape, dt=F32):
        t = nc.dram_tensor("dbg_" + name, list(shape), dt, kind="ExternalOutput").ap()
        dbg_out[name] = t
        return t

    scr_hT = nc.dram_tensor("scr_hT", [128, 8, 128 + TQ], BF16, kind="Internal").ap()
    scr_ya = nc.dram_tensor("scr_ya", [128, 8, TQ], BF16, kind="Internal").ap()
    scr_h1 = nc.dram_tensor("scr_h1", [TQ, D], F32, kind="Internal").ap()
    scr_h = nc.dram_tensor("scr_h", [TQ, D], F32, kind="Internal").ap()
    scr_h1T = nc.dram_tensor("scr_h1T", [128, 8, TQ], BF16, kind="Internal").ap()

    def alloc(stack, name, shape, dt):
        return stack.enter_context(nc.sbuf_tensor(name, list(shape), dt))

    top = ExitStack()
    ident_b = alloc(top, "ident_b", [128, 128], BF16)
    ident_f = alloc(top, "ident_f", [128, 128], F32)
    ones_b = alloc(top, "ones_b", [128, 512], BF16)
    cv = alloc(top, "cv", [128, 8], F32)
    eps_t = alloc(top, "eps_t", [128, 1], F32)
    st = alloc(top, "st", [128, 16], F32)
    junk = alloc(top, "junk", [128, D], F32)
    S.dma("pool", ident_b[:], C["ident"])
    S.dma("sp", ident_f[:], C["ident"])
    S.dma("sp", cv[:], cvec)
    S.memset("dve", ones_b[:], 1.0)
    S.memset("dve", eps_t[:], LN_EPS)

    ps = [nc.alloc_psum_tensor("ps%d" % i, [128, 512], F32) for i in range(7)]
    psT = nc.alloc_psum_tensor("psT", [128, 1024], BF16)


    def ln_tok(x_ap, g_rep, b_rep, out_ap, n, tmp_ap):
        S.act(junk[:, 0:n], x_ap, AF.Identity, accum_out=st[:, 0:1])
        S.act(junk[:, 0:n], x_ap, AF.Square, accum_out=st[:, 1:2])
        S.ts("dve", st[:, 2:3], st[:, 0:1], 1.0 / n, None, ALU.mult)
        S.tt("dve", st[:, 3:4], st[:, 2:3], st[:, 2:3], ALU.mult)
        S.stt("dve", st[:, 4:5], st[:, 1:2], 1.0 / n, st[:, 3:4], ALU.mult, ALU.subtract)
        S.act(st[:, 5:6], st[:, 4:5], AF.Sqrt, bias=eps_t[:, 0:1])
        S.recip(st[:, 6:7], st[:, 5:6])
        S.stt("dve", st[:, 7:8], st[:, 2:3], -1.0, st[:, 6:7], ALU.mult, ALU.mult)
        S.act(tmp_ap, x_ap, AF.Identity, bias=st[:, 7:8], scale=st[:, 6:7])
        S.tt("dve", tmp_ap, tmp_ap, g_rep, ALU.mult)
        S.tt("dve", out_ap, tmp_ap, b_rep, ALU.add)

    w_in_k = w_in.rearrange("(kc p) n -> p kc n", p=128)

    def load_col(dst, row_ap):
        S.dma("sp", dst, row_ap.rearrange("(c p) -> p c", p=128), allow_slow_non_contiguous=True)

    keys = ExitStack()
    ckvT = alloc(keys, "ckvT", [128, SEQ], BF16)
    ckv1 = alloc(keys, "ckv1", [128, NKT, 129], BF16)
    kidxT = alloc(keys, "kidxT", [128, SEQ], BF16)
    S.memset("pool", ckv1[:, :, 128:129], 1.0)
    nkt = lim.get("nkt", NKT)
    with ExitStack() as pk:
        g0_rep = alloc(pk, "g0_rep", [128, D], F32)
        b0_rep = alloc(pk, "b0_rep", [128, D], F32)
        S.dma("sp", g0_rep[:], ln0_g.partition_broadcast(128))
        S.dma("sp", b0_rep[:], ln0_b.partition_broadcast(128))
        wkv = alloc(pk, "wkv", [128, 8, 256], BF16)
        S.dma("pool", wkv[:, :, 0:128], w_in_k[:, :, OFF_CKV:OFF_CKV + 128])
        for r in range(4):
            S.dma("pool", wkv[:, :, 128 + 32 * r:160 + 32 * r], w_in_k[:, :, OFF_KIDX:OFF_KIDX + 32])
        bkv = alloc(pk, "bkv", [1, 256], BF16)
        S.dma("pool", bkv[:, 0:128], b_in[:, OFF_CKV:OFF_CKV + 128])
        for r in range(4):
            S.dma("pool", bkv[:, 128 + 32 * r:160 + 32 * r], b_in[:, OFF_KIDX:OFF_KIDX + 32])
        gkv_rep = alloc(pk, "gkv_rep", [128, 128], F32)
        S.dma("sp", gkv_rep[:], kv_norm_g.partition_broadcast(128))
        xbuf = [alloc(pk, "xbuf%d" % i, [128, D], F32) for i in range(2)]
        xn = alloc(pk, "xn", [128, D], F32)
        hb = alloc(pk, "hb", [128, D], BF16)
        hf = alloc(pk, "hf", [128, D], F32)
        hTt = [alloc(pk, "hTt%d" % i, [128, 8, 128], BF16) for i in range(2)]
        kx = alloc(pk, "kx", [128, 128], BF16)
        for kt in range(nkt):
            xt = xbuf[kt % 2]
            S.dma("sp", xt[:], xk[kt * 128:(kt + 1) * 128, :])
            if kt >= 48:
                ln_tok(xt[:], g0_rep[:], b0_rep[:], hf[:], D, xn[:])
                S.dma("sp", scr_h[(kt - 48) * 128:(kt - 47) * 128, :], hf[:])
                S.cp("act", hb[:], hf[:])
            else:
                ln_tok(xt[:], g0_rep[:], b0_rep[:], hb[:], D, xn[:])
            for kc in range(8):
                S.tr(psT[:, kc * 128:(kc + 1) * 128], hb[:, kc * 128:(kc + 1) * 128], ident_b[:])
            ht = hTt[kt % 2]
            S.cp("act", ht[:].rearrange("p a b -> p (a b)"), psT[:])
            if kt >= 47:
                S.dma("sp", scr_hT[:, :, (kt - 47) * 128:(kt - 46) * 128], ht[:])
            pk_ = ps[kt % 2]
            for kc in range(8):
                S.mm(pk_[:, 0:256], ht[:, kc, :], wkv[:, kc, :], start=(kc == 0), stop=False)
            S.mm(pk_[:, 0:256], ones_b[0:1, 0:128], bkv[0:1, :], start=False, stop=True)
            S.act(junk[:, 0:128], pk_[:, 0:128], AF.Square, accum_out=st[:, 8:9])
            S.ts("dve", st[:, 9:10], st[:, 8:9], 1.0 / 128, None, ALU.mult)
            S.act(st[:, 10:11], st[:, 9:10], AF.Sqrt, bias=eps_t[:, 0:1])
            S.recip(st[:, 11:12], st[:, 10:11])
            S.stt("dve", ckv1[:, kt, 0:128], pk_[:, 0:128], st[:, 11:12], gkv_rep[:], ALU.mult, ALU.mult)
            S.cp("act", kx[:], pk_[:, 128:256])
            S.tr(psT[:, 0:128], ckv1[:, kt, 0:128], ident_b[:])
            S.tr(psT[:, 128:256], kx[:], ident_b[:])
            S.cp("act", ckvT[:, kt * 128:(kt + 1) * 128], psT[:, 0:128])
            S.cp("act", kidxT[:, kt * 128:(kt + 1) * 128], psT[:, 128:256])

    S.barrier()
    if "keys" in dbg:
        d1 = dbg_tensor("ckvT", [128, SEQ], BF16)
        d2 = dbg_tensor("kidxT", [128, SEQ], BF16)
        S.dma("sp", d1[:, 0:nkt * 128], ckvT[:, 0:nkt * 128])
        S.dma("sp", d2[:, 0:nkt * 128], kidxT[:, 0:nkt * 128])

    if stage <= 1:
        S.finish()
        return nc, dbg_out, S

    class _Stop(Exception):
        pass

    def ckpt(k):
        if lim.get("a_stop", 99) <= k:
            raise _Stop()

    try:
      with ExitStack() as pa:
          E_b = alloc(pa, "E_b", [128, 1024], BF16); S.dma("pool", E_b[:], C["E"])
          cd_b = alloc(pa, "cd_b", [128, 1024], BF16); S.dma("pool", cd_b[:], C["cd"])
          lpos_b = alloc(pa, "lpos_b", [48, SEQ], BF16); S.dma("pool", lpos_b[:], C["lpos"])
          ldiag_b = alloc(pa, "ldiag_b", [48, NT * 128], BF16); S.dma("pool", ldiag_b[:], C["ldiag"])
          Rb = [alloc(pa, "Rb%d" % k, [48, 1024], BF16) for k in range(2)]
          for k in range(2):
              S.dma("pool", Rb[k][:], C["rb"])
          bd_b = alloc(pa, "bd_b", [16, 1024], BF16); S.dma("pool", bd_b[:], C["bd"])
          dg = alloc(pa, "dg", [128, 128], F32); S.dma("sp", dg[:], C["dg"])
          slope8 = alloc(pa, "slope8", [128, 8], F32); S.dma("sp", slope8[:], C["slope8"])
          jpos = alloc(pa, "jpos", [128, 128], F32); S.dma("sp", jpos[:], C["jpos"])
          tlc = alloc(pa, "tlc", [128, 1], F32); S.dma("sp", tlc[:], C["tl"])
          half = alloc(pa, "half", [128, NITER], F32); S.dma("sp", half[:], C["half"])
          wq = alloc(pa, "wq", [128, 8, 776], BF16)
          bq = alloc(pa, "bq", [1, 776], BF16)
          for (dst0, src0, n) in ((0, OFF_QA, 512), (512, OFF_QIDX, 256), (768, OFF_WIDX, 8)):
              S.dma("pool", wq[:, :, dst0:dst0 + n], w_in_k[:, :, src0:src0 + n])
              S.dma("pool", bq[:, dst0:dst0 + n], b_in[:, src0:src0 + n])
          wuk_b = alloc(pa, "wuk_b", [128, 8, 64], BF16)
          S.dma("pool", wuk_b[:], w_uk.rearrange("h r d -> r h d"))
          wuv = alloc(pa, "wuv", [128, 8, 64], BF16)
          S.dma("pool", wuv[:], w_uv.rearrange("h r d -> r h d"))
          woa = alloc(pa, "woa", [64, 8, D], BF16)
          S.dma("pool", woa[:], w_o_a.rearrange("(h d) n -> d h n", d=64))
          wukT = alloc(pa, "wukT", [64, 8, 128], BF16)
          for hh in range(8):
              S.tr(psT[0:64, hh * 128:(hh + 1) * 128], wuk_b[:, hh, :], ident_b[:])
          S.cp("act", wukT[:].rearrange("p a b -> p (a b)"), psT[0:64, :])

          ckpt(1)
          acc = alloc(pa, "acc", [128, SEQ], F32)
          maskb = [alloc(pa, "maskb%d" % k, [128, SEQ], BF16) for k in range(2)]
          hTi = [alloc(pa, "hTi%d" % k, [128, 8, 128], BF16) for k in range(2)]
          qa_sb = alloc(pa, "qa_sb", [64, 8, 128], BF16)
          qlat = [alloc(pa, "qlat%d" % k, [128, 1024], BF16) for k in range(2)]
          qi = alloc(pa, "qi", [32, 8, 128], BF16)
          wabs = alloc(pa, "wabs", [128, 8], F32)
          sgn = alloc(pa, "sgn", [128, 8], F32)
          rt = [alloc(pa, "rt%d" % i, [128, 512], F32) for i in range(2)]
          PT = [alloc(pa, "PT%d" % i, [128, 512], BF16) for i in range(2)]
          sm = alloc(pa, "sm", [128, 16], F32)
          steps = alloc(pa, "steps", [128, NITER], F32)
          r1 = alloc(pa, "r1", [128, 128], F32)
          v8 = alloc(pa, "v8", [128, 8], F32)
          vhf = alloc(pa, "vhf", [128, 8], F32)
          vhl = alloc(pa, "vhl", [128, 16], BF16)
          vT = alloc(pa, "vT", [16, 128], BF16)
          rden = alloc(pa, "rden", [128, 8], F32)
          olat = alloc(pa, "olat", [128, 8, 128], BF16)
          olatT = alloc(pa, "olatT", [128, 1024], BF16)
          z_sb = qa_sb
          yat = olat
          Amax, Wd, lo, mid, cnt, tmp1, m1, ddv = [sm[:, k:k + 1] for k in range(8)]

          def po(hh):
              bank = ps[2 + hh // 3]
              k = hh % 3
              return bank[:, k * 129:(k + 1) * 129]

          ntl = lim.get("nt", NT)

          def qproj(i):
              p = i % 2
              hT_ = hTi[p]
              S.dma("sp", hT_[:], scr_hT[:, :, 128 + i * 128:128 + (i + 1) * 128])
              for hh in range(8):
                  o = ps[5 + hh // 4][0:64, (hh % 4) * 128:(hh % 4 + 1) * 128]
                  for kc in range(8):
                      S.mm(o, wq[:, kc, hh * 64:(hh + 1) * 64], hT_[:, kc, :], start=(kc == 0), stop=False)
                  S.mm(o, bq[0:1, hh * 64:(hh + 1) * 64], ones_b[0:1, 0:128], start=False, stop=True)
              S.cp("act", qa_sb[:, 0:4, :].rearrange("p a b -> p (a b)"), ps[5][0:64, :])
              S.cp("act", qa_sb[:, 4:8, :].rearrange("p a b -> p (a b)"), ps[6][0:64, :])
              for hh in range(8):
                  o = ps[5 + hh // 4][:, (hh % 4) * 128:(hh % 4 + 1) * 128]
                  S.mm(o, wukT[:, hh, :], qa_sb[:, hh, :], start=True, stop=True)
              S.cp("act", qlat[p][:, 0:512], ps[5][:])
              S.cp("act", qlat[p][:, 512:1024], ps[6][:])
              for hh in range(8):
                  o = ps[5 + hh // 4][0:32, (hh % 4) * 128:(hh % 4 + 1) * 128]
                  c0 = 512 + hh * 32
                  for kc in range(8):
                      S.mm(o, wq[:, kc, c0:c0 + 32], hT_[:, kc, :], start=(kc == 0), stop=False)
                  S.mm(o, bq[0:1, c0:c0 + 32], ones_b[0:1, 0:128], start=False, stop=True)
              S.cp("act", qi[:, 0:4, :].rearrange("p a b -> p (a b)"), ps[5][0:32, :])
              S.cp("act", qi[:, 4:8, :].rearrange("p a b -> p (a b)"), ps[6][0:32, :])
              for kc in range(8):
                  S.mm(ps[6][:, 0:8], hT_[:, kc, :], wq[:, kc, 768:776], start=(kc == 0), stop=False)
              S.mm(ps[6][:, 0:8], ones_b[0:1, 0:128], bq[0:1, 768:776], start=False, stop=True)
              S.act(wabs[:], ps[6][:, 0:8], AF.Abs, scale=IDX_SCALE)
              S.ts("dve", sgn[:], ps[6][:, 0:8], 0.0, 2.0, ALU.is_ge, ALU.mult)
              S.ts("dve", sgn[:], sgn[:], -1.0, None, ALU.add)

          def scores(i):
              it = 48 + i
              L = (it + 1) * 128
              nch = (L + 511) // 512
              n = 0
              for hh in range(8):
                  for sc in range(nch):
                      c0 = sc * 512
                      c1 = min(L, c0 + 512)
                      w = c1 - c0
                      pss = ps[5 + n % 2]
                      rtt = rt[n % 2]
                      n += 1
                      S.mm(pss[:, 0:w], qi[:, hh, :], kidxT[0:32, c0:c1], start=True, stop=True)
                      S.act(rtt[:, 0:w], pss[:, 0:w], AF.Relu, scale=wabs[:, hh:hh + 1])
                      if hh == 0:
                          S.ts("dve", acc[:, c0:c1], rtt[:, 0:w], sgn[:, 0:1], None, ALU.mult)
                      else:
                          S.stt("dve", acc[:, c0:c1], rtt[:, 0:w], sgn[:, hh:hh + 1], acc[:, c0:c1],
                                ALU.mult, ALU.add)

          def bisect(i):
              p = i % 2
              it = 48 + i
              L = (it + 1) * 128
              base = float(6144 + 128 * i)
              mk = maskb[p]
              S.red(Amax, acc[:, 0:L], ALU.max)
              S.red(tmp1, acc[:, 0:L], ALU.min)
              S.stt("dve", Amax, tmp1, -1.0, Amax, ALU.mult, ALU.max)
              for q in range(3):
                  S.ts("dve", acc[:, q * TQ:(q + 1) * TQ], acc[:, q * TQ:(q + 1) * TQ], cv[:, q:q + 1], None, ALU.add)
              S.tt("dve", acc[:, it * 128:L], acc[:, it * 128:L], dg[:], ALU.add)
              S.ts("dve", Wd, Amax, 2.002, 2e-6, ALU.mult, ALU.add)
              S.ts("dve", lo, Wd, -0.5, None, ALU.mult)
              S.ts("dve", steps[:], half[:], Wd, None, ALU.mult)
              for k in range(NITER):
                  S.tt("dve", mid, lo, steps[:, k:k + 1], ALU.add)
                  S.ts("dve", mk[:, 0:L], acc[:, 0:L], mid, None, ALU.is_ge, op1=ALU.add, accum_out=cnt)
                  S.stt("dve", tmp1, cnt, 255.5, steps[:, k:k + 1], ALU.is_ge, ALU.mult)
                  S.tt("dve", lo, lo, tmp1, ALU.add)
              S.ts("dve", mk[:, 0:L], acc[:, 0:L], lo, MB, ALU.is_lt, ALU.mult)
              nh = L // 64
              S.red(r1[:, 0:nh], mk[:, 0:L].rearrange("p (a b) -> p a b", b=64), ALU.max)
              S.tt("dve", r1[:, 0:nh], r1[:, 0:nh], jpos[:, 0:nh], ALU.add)
              S.red(m1, r1[:, 0:nh], ALU.max)
              S.ts("dve", ddv, m1, -1.0, tlc[:, 0:1], ALU.mult, ALU.add)
              S.ts("dve", ddv, ddv, base, 0.0, ALU.add, ALU.max)
              S.ts("dve", ddv, ddv, tlc[:, 0:1], base, ALU.subtract, ALU.subtract)
              S.ts("dve", v8[:], slope8[:], ddv, None, ALU.mult)
              S.cp("dve", vhl[:, 0:8], v8[:])
              S.cp("dve", vhf[:], vhl[:, 0:8])
              S.tt("dve", vhl[:, 8:16], v8[:], vhf[:], ALU.subtract)
              S.tr(psT[0:16, 0:128], vhl[:], ident_b[:])
              S.cp("act", vT[:], psT[0:16, 0:128])
              S.tt("dve", Rb[p][0:16, :].rearrange("p (h t) -> p h t", h=8),
                   vT[:].unsqueeze(1).to_broadcast([16, 8, 128]),
                   bd_b[:].rearrange("p (h t) -> p h t", h=8), ALU.mult)
              if "dsa" in dbg and i == 0:
                  dd1 = dbg_tensor("maskb", [128, SEQ], F32)
                  S.dma("pool", dd1[:, 0:L], mk[:, 0:L])
                  dd2 = dbg_tensor("sm", [128, 16], F32)
                  S.dma("sp", dd2[:, 0:8], sm[:, 0:8])
                  dd3 = dbg_tensor("acc", [128, SEQ], F32)
                  S.dma("sp", dd3[:, 0:L], acc[:, 0:L])
                  dd4 = dbg_tensor("qlat", [128, 1024], BF16)
                  S.dma("sp", dd4, qlat[p][:])

          def attn(i):
              p = i % 2
              it = 48 + i
              mk = maskb[p]
              first_in_bank = {0: True, 3: True, 6: True}
              chunks = [(j, c) for j in range(it + 1) for c in range(2)]

              def logits(n):
                  j, c = chunks[n]
                  pl = ps[n % 2]
                  ptt = PT[n % 2]
                  cs = slice(c * 512, (c + 1) * 512)
                  S.mm(pl[:], ckvT[:, j * 128:(j + 1) * 128], qlat[p][:, cs], start=True, stop=False)
                  if j < it:
                      S.mm(pl[:], lpos_b[0:48, j * 128:(j + 1) * 128], Rb[p][0:48, cs], start=False, stop=False)
                  else:
                      S.mm(pl[:], ldiag_b[0:48, i * 128:(i + 1) * 128], Rb[p][0:48, cs], start=False, stop=False)
                      S.mm(pl[:], ident_b[:], cd_b[:, cs], start=False, stop=False)
                  S.mm(pl[:], mk[:, j * 128:(j + 1) * 128], E_b[:, cs], start=False, stop=True)
                  S.act(ptt[:], pl[:], AF.Exp, scale=0.125)

              def pv(n):
                  j, c = chunks[n]
                  ptt = PT[n % 2]
                  for h4 in range(4):
                      hh = 4 * c + h4
                      S.mm(po(hh), ptt[:, h4 * 128:(h4 + 1) * 128], ckv1[:, j, :],
                           start=(j == 0 and hh in first_in_bank), stop=(j == it))

              logits(0)
              for n in range(len(chunks)):
                  if n + 1 < len(chunks):
                      logits(n + 1)
                  pv(n)

          def fin(i):
              for hh in range(8):
                  S.recip(rden[:, hh:hh + 1], po(hh)[:, 128:129])
              for hh in range(8):
                  S.act(olat[:, hh, :], po(hh)[:, 0:128], AF.Identity, scale=rden[:, hh:hh + 1])
              for hh in range(8):
                  S.tr(psT[:, hh * 128:(hh + 1) * 128], olat[:, hh, :], ident_b[:])
              S.cp("act", olatT[:], psT[:])
              for hh in range(8):
                  o = ps[5 + hh // 4][0:64, (hh % 4) * 128:(hh % 4 + 1) * 128]
                  S.mm(o, wuv[:, hh, :], olatT[:, hh * 128:(hh + 1) * 128], start=True, stop=True)
              S.cp("act", z_sb[:, 0:4, :].rearrange("p a b -> p (a b)"), ps[5][0:64, :])
              S.cp("act", z_sb[:, 4:8, :].rearrange("p a b -> p (a b)"), ps[6][0:64, :])
              for dc in range(8):
                  o = ps[5 + dc // 4][:, (dc % 4) * 128:(dc % 4 + 1) * 128]
                  for hh in range(8):
                      S.mm(o, woa[:, hh, dc * 128:(dc + 1) * 128], z_sb[:, hh, :], start=(hh == 0), stop=(hh == 7))
              S.cp("act", yat[:, 0:4, :].rearrange("p a b -> p (a b)"), ps[5][:])
              S.cp("act", yat[:, 4:8, :].rearrange("p a b -> p (a b)"), ps[6][:])
              S.dma("sp", scr_ya[:, :, i * 128:(i + 1) * 128], yat[:])

          qproj(0)
          scores(0)
          bisect(0)
          for i in range(ntl):
              if i + 1 < ntl:
                  qproj(i + 1)
                  scores(i + 1)
              if lim.get("bar1", False):
                  S.barrier()
              attn(i)
              if i + 1 < ntl:
                  bisect(i + 1)
              S.barrier()
              fin(i)
          if "dsa" in dbg:
              dd5 = dbg_tensor("ya", [128, 8, TQ], BF16)
              S.dma("sp", dd5[:, :, 0:ntl * 128], scr_ya[:, :, 0:ntl * 128])
    except _Stop:
        S.finish()
        return nc, dbg_out, S
    keys.close()
    S.barrier()

    if stage <= 2:
        S.finish()
        return nc, dbg_out, S

    TG = 256
    C_SCALE = 128 ** -0.5
    with ExitStack() as pm:
        g1_rep = alloc(pm, "g1_rep", [128, D], F32); S.dma("sp", g1_rep[:], ln1_g.partition_broadcast(128))
        b1_rep = alloc(pm, "b1_rep", [128, D], F32); S.dma("sp", b1_rep[:], ln1_b.partition_broadcast(128))
        onesf = alloc(pm, "onesf", [128, 128], F32); S.memset("dve", onesf[:], 1.0 / 512)
        wglu = alloc(pm, "wglu", [128, 8, 1024], BF16); S.dma("pool", wglu[:], w_in_k[:, :, OFF_GLU:OFF_GLU + 1024])
        wqc = alloc(pm, "wqc", [128, 8, 512], BF16); S.dma("pool", wqc[:], w_in_k[:, :, OFF_QC:OFF_QC + 512])
        wgate = alloc(pm, "wgate", [128, 8, 3072], BF16)
        for k in range(3):
            S.dma("pool", wgate[:, :, k * 1024:(k + 1) * 1024], w_in_k[:, :, OFF_GATE + k * 1024:OFF_GATE + (k + 1) * 1024])
        wpw2 = alloc(pm, "wpw2", [128, 4, D], BF16); S.dma("pool", wpw2[:], w_pw2.rearrange("(c p) n -> p c n", p=128))
        woc = alloc(pm, "woc", [128, 4, D], BF16); S.dma("pool", woc[:], w_o_c.rearrange("(c p) n -> p c n", p=128))
        wout = alloc(pm, "wout", [128, 8, D], BF16); S.dma("pool", wout[:], w_out.rearrange("(c p) n -> p c n", p=128))
        bout = alloc(pm, "bout", [1, D], BF16); S.dma("pool", bout[:], b_out)
        bglu = alloc(pm, "bglu", [128, 8], F32); load_col(bglu[:], b_in[0, OFF_GLU:OFF_GLU + 1024])
        bqc = alloc(pm, "bqc", [128, 4], F32); load_col(bqc[:], b_in[0, OFF_QC:OFF_QC + 512])
        bgate = alloc(pm, "bgate", [128, 24], F32); load_col(bgate[:], b_in[0, OFF_GATE:OFF_GATE + 3072])
        bpw2 = alloc(pm, "bpw2", [128, 8], F32); load_col(bpw2[:], b_pw2[0, :])
        bdw = alloc(pm, "bdw", [128, 4], F32); load_col(bdw[:], b_dw[0, :])
        cg = alloc(pm, "cg", [128, 4], F32); load_col(cg[:], conv_ln_g[0, :])
        cb = alloc(pm, "cb", [128, 4], F32); load_col(cb[:], conv_ln_b[0, :])
        wdw_r = alloc(pm, "wdw_r", [31, 512], F32); S.dma("sp", wdw_r[:], w_dw)
        wdw = alloc(pm, "wdw", [128, 4, 32], F32)
        for ch in range(4):
            S.tr(ps[0][:, ch * 32:ch * 32 + 31], wdw_r[0:31, ch * 128:(ch + 1) * 128], ident_f[0:31, 0:31])
        for ch in range(4):
            S.cp("act", wdw[:, ch, 0:31], ps[0][:, ch * 32:ch * 32 + 31])
        kT_sb = alloc(pm, "kT_sb", [128, 4, 256], BF16)
        v_sb = alloc(pm, "v_sb", [128, 2, 512], BF16)
        with ExitStack() as pmem:
            wmk = alloc(pmem, "wmk", [128, 8, 512], BF16); S.dma("pool", wmk[:], w_mem_k.rearrange("(c p) n -> p c n", p=128))
            wmv = alloc(pmem, "wmv", [128, 8, 512], BF16); S.dma("pool", wmv[:], w_mem_v.rearrange("(c p) n -> p c n", p=128))
            mem_b = alloc(pmem, "mem_b", [128, 2, D], BF16); S.dma("pool", mem_b[:], memb.rearrange("(m p) n -> p m n", p=128))
            memT = alloc(pmem, "memT", [128, 8, 256], BF16)
            for mt in range(2):
                for kc in range(8):
                    S.tr(psT[:, kc * 128:(kc + 1) * 128], mem_b[:, mt, kc * 128:(kc + 1) * 128], ident_b[:])
                S.cp("act", memT[:, :, mt * 128:(mt + 1) * 128], psT[:].rearrange("p (a b) -> p a b", a=8))
            for hh in range(4):
                o = ps[1][:, 0:256]
                for kc in range(8):
                    S.mm(o, wmk[:, kc, hh * 128:(hh + 1) * 128], memT[:, kc, :], start=(kc == 0), stop=(kc == 7))
                S.cp("act", kT_sb[:, hh, :], o)
            for mt in range(2):
                o = ps[2][:]
                for kc in range(8):
                    S.mm(o, memT[:, kc, mt * 128:(mt + 1) * 128], wmv[:, kc, :], start=(kc == 0), stop=(kc == 7))
                S.cp("act", v_sb[:, mt, :], o)
        S.barrier()

        hTg = alloc(pm, "hTg", [128, 8, TG], BF16)
        hTh = alloc(pm, "hTh", [128, 8, 128], BF16)
        ub = alloc(pm, "ub", [128, 4, 30 + TG], F32)
        uh = alloc(pm, "uh", [128, 4, 128], F32)
        sg = alloc(pm, "sg", [128, TG], F32)
        vv = alloc(pm, "vv", [128, 4, TG], F32)
        mean_sb = alloc(pm, "mean_sb", [128, TG], F32)
        rstd_sb = alloc(pm, "rstd_sb", [128, TG], F32)
        xc = alloc(pm, "xc", [128, TG], F32)
        s_b = alloc(pm, "s_b", [128, 4, TG], BF16)
        gt = alloc(pm, "gt", [128, TG], F32)
        tmpm = alloc(pm, "tmpm", [128, TG], F32)
        mixed = alloc(pm, "mixed", [128, 8, TG], F32)
        yag = alloc(pm, "yag", [128, 8, TG], BF16)
        mixed_b = yag
        qc_b = alloc(pm, "qc_b", [128, TG], BF16)
        PTc = alloc(pm, "PTc", [128, 2, TG], BF16)
        rdc = alloc(pm, "rdc", [128, TG], F32)
        oc = alloc(pm, "oc", [128, 4, TG], BF16)
        hres = alloc(pm, "hres", [128, D], F32)
        pre = alloc(pm, "pre", [128, D], F32)
        lnt = alloc(pm, "lnt", [128, D], F32)
        h1t = alloc(pm, "h1t", [128, D], F32)
        h1b = alloc(pm, "h1b", [128, D], BF16)
        h1Tt = alloc(pm, "h1Tt", [128, 8, 128], BF16)

        def glu(hsrc, n, dst):
            for ch in range(4):
                pa_, pg_ = ps[0][:, 0:n], ps[1][:, 0:n]
                for kc in range(8):
                    S.mm(pa_, wglu[:, kc, ch * 128:(ch + 1) * 128], hsrc[:, kc, :], start=(kc == 0), stop=(kc == 7))
                for kc in range(8):
                    S.mm(pg_, wglu[:, kc, 512 + ch * 128:512 + (ch + 1) * 128], hsrc[:, kc, :], start=(kc == 0), stop=(kc == 7))
                S.act(sg[:, 0:n], pg_, AF.Sigmoid, bias=bglu[:, 4 + ch:5 + ch])
                S.stt("dve", dst(ch), pa_, bglu[:, ch:ch + 1], sg[:, 0:n], ALU.add, ALU.mult)

        def gate_chunk(k, dc):
            pg_ = ps[2 + dc % 2][:, 0:TG]
            for kc in range(8):
                S.mm(pg_, wgate[:, kc, k * 1024 + dc * 128:k * 1024 + (dc + 1) * 128], hTg[:, kc, :],
                     start=(kc == 0), stop=(kc == 7))
            S.act(gt[:], pg_, AF.Sigmoid, bias=bgate[:, k * 8 + dc:k * 8 + dc + 1])

        ntg = lim.get("ntg", TQ // TG)
        for tg in range(ntg):
            S.dma("sp", hTg[:], scr_hT[:, :, 128 + tg * TG:128 + (tg + 1) * TG])
            S.dma("sp", yag[:], scr_ya[:, :, tg * TG:(tg + 1) * TG])
            if tg == 0:
                S.dma("sp", hTh[:], scr_hT[:, :, 0:128])
                glu(hTh, 128, lambda ch: uh[:, ch, :])
                for ch in range(4):
                    S.ts("dve", ub[:, ch, 0:30], uh[:, ch, 98:128], cv[:, 3:4], None, ALU.mult)
            else:
                for ch in range(4):
                    S.cp("dve", xc[:, 0:30], ub[:, ch, TG:TG + 30])
                    S.cp("dve", ub[:, ch, 0:30], xc[:, 0:30])
            glu(hTg, TG, lambda ch: ub[:, ch, 30:30 + TG])
            for k in range(31):
                for ch in range(4):
                    if k == 0:
                        S.ts("dve", vv[:, ch, :], ub[:, ch, 0:TG], wdw[:, ch, 0:1], bdw[:, ch:ch + 1], ALU.mult, ALU.add)
                    else:
                        S.stt("dve", vv[:, ch, :], ub[:, ch, k:k + TG], wdw[:, ch, k:k + 1], vv[:, ch, :], ALU.mult, ALU.add)
            for ch in range(4):
                S.mm(ps[2][:, 0:TG], onesf[:], vv[:, ch, :], start=(ch == 0), stop=(ch == 3))
            for ch in range(4):
                S.act(xc[:], vv[:, ch, :], AF.Square)
                S.mm(ps[3][:, 0:TG], onesf[:], xc[:], start=(ch == 0), stop=(ch == 3))
            S.cp("act", mean_sb[:], ps[2][:, 0:TG])
            S.tt("dve", xc[:], mean_sb[:], mean_sb[:], ALU.mult)
            S.tt("dve", rstd_sb[:], ps[3][:, 0:TG], xc[:], ALU.subtract)
            S.act(rstd_sb[:], rstd_sb[:], AF.Sqrt, bias=eps_t[:, 0:1])
            S.recip(rstd_sb[:], rstd_sb[:])
            for ch in range(4):
                S.tt("dve", xc[:], vv[:, ch, :], mean_sb[:], ALU.subtract)
                S.tt("dve", xc[:], xc[:], rstd_sb[:], ALU.mult)
                S.act(s_b[:, ch, :], xc[:], AF.Silu, scale=cg[:, ch:ch + 1], bias=cb[:, ch:ch + 1])
            for dc in range(8):
                gate_chunk(0, dc)
                S.tt("dve", mixed[:, dc, :], yag[:, dc, :], gt[:], ALU.mult)
            for dc in range(8):
                pb_ = ps[dc % 2][:, 0:TG]
                for ch in range(4):
                    S.mm(pb_, wpw2[:, ch, dc * 128:(dc + 1) * 128], s_b[:, ch, :], start=(ch == 0), stop=(ch == 3))
                gate_chunk(1, dc)
                S.stt("dve", tmpm[:], pb_, bpw2[:, dc:dc + 1], gt[:], ALU.add, ALU.mult)
                S.tt("dve", mixed[:, dc, :], mixed[:, dc, :], tmpm[:], ALU.add)
            for hh in range(4):
                pq_ = ps[4][:, 0:TG]
                for kc in range(8):
                    S.mm(pq_, wqc[:, kc, hh * 128:(hh + 1) * 128], hTg[:, kc, :], start=(kc == 0), stop=(kc == 7))
                S.act(qc_b[:], pq_, AF.Identity, bias=bqc[:, hh:hh + 1])
                for mt in range(2):
                    S.mm(ps[5][:, 0:TG], kT_sb[:, hh, mt * 128:(mt + 1) * 128], qc_b[:], start=True, stop=True)
                    S.act(PTc[:, mt, :], ps[5][:, 0:TG], AF.Exp, scale=C_SCALE)
                for mt in range(2):
                    S.mm(ps[6][:, 0:TG], v_sb[:, mt, hh * 128:(hh + 1) * 128], PTc[:, mt, :], start=(mt == 0), stop=(mt == 1))
                for mt in range(2):
                    S.mm(ps[5][:, 0:TG], ones_b[:, 0:128], PTc[:, mt, :], start=(mt == 0), stop=(mt == 1))
                S.recip(rdc[:], ps[5][:, 0:TG])
                S.tt("dve", oc[:, hh, :], ps[6][:, 0:TG], rdc[:], ALU.mult)
            for dc in range(8):
                pc_ = ps[dc % 2][:, 0:TG]
                for hh in range(4):
                    S.mm(pc_, woc[:, hh, dc * 128:(dc + 1) * 128], oc[:, hh, :], start=(hh == 0), stop=(hh == 3))
                gate_chunk(2, dc)
                S.tt("dve", tmpm[:], pc_, gt[:], ALU.mult)
                S.tt("dve", mixed[:, dc, :], mixed[:, dc, :], tmpm[:], ALU.add)
            for dc in range(8):
                S.cp("act", mixed_b[:, dc, :], mixed[:, dc, :])
            for t4 in range(TG // 128):
                ti = tg * (TG // 128) + t4
                S.dma("sp", hres[:], scr_h[ti * 128:(ti + 1) * 128, :])
                for dh in range(2):
                    po_ = ps[dh][:]
                    for dc in range(8):
                        S.mm(po_, mixed_b[:, dc, t4 * 128:(t4 + 1) * 128], wout[:, dc, dh * 512:(dh + 1) * 512],
                             start=(dc == 0), stop=False)
                    S.mm(po_, ones_b[0:1, 0:128], bout[0:1, dh * 512:(dh + 1) * 512], start=False, stop=True)
                    S.stt("dve", pre[:, dh * 512:(dh + 1) * 512], hres[:, dh * 512:(dh + 1) * 512], DN_ALPHA, po_,
                          ALU.mult, ALU.add)
                ln_tok(pre[:], g1_rep[:], b1_rep[:], h1t[:], D, lnt[:])
                S.dma("sp", scr_h1[ti * 128:(ti + 1) * 128, :], h1t[:])
                S.cp("act", h1b[:], h1t[:])
                for kc in range(8):
                    S.tr(psT[:, kc * 128:(kc + 1) * 128], h1b[:, kc * 128:(kc + 1) * 128], ident_b[:])
                S.cp("act", h1Tt[:].rearrange("p a b -> p (a b)"), psT[:])
                S.dma("sp", scr_h1T[:, :, ti * 128:(ti + 1) * 128], h1Tt[:])
        if "mix" in dbg:
            dm = dbg_tensor("h1", [TQ, D], F32)
            S.dma("sp", dm[0:ntg * TG, :], scr_h1[0:ntg * TG, :])
    S.barrier()
    if stage <= 3:
        S.finish()
        return nc, dbg_out, S

    with ExitStack() as pe_:
        g2_rep = alloc(pe_, "g2_rep", [128, D], F32); S.dma("sp", g2_rep[:], ln2_g.partition_broadcast(128))
        b2_rep = alloc(pe_, "b2_rep", [128, D], F32); S.dma("sp", b2_rep[:], ln2_b.partition_broadcast(128))
        h1T = alloc(pe_, "h1T", [128, 8, TQ], BF16)
        S.dma("sp", h1T[:], scr_h1T)
        gates = alloc(pe_, "gates", [128, NT, 32], F32)
        accm = alloc(pe_, "accm", [128, NT, D], F32)
        m8 = alloc(pe_, "m8", [128, 8], F32)
        lg = alloc(pe_, "lg", [128, 32], F32)
        ex = alloc(pe_, "ex", [128, 32], F32)
        sm2 = alloc(pe_, "sm2", [128, 4], F32)
        gT = alloc(pe_, "gT", [32, TQ], BF16)
        gb = alloc(pe_, "gb", [128, 32], BF16)
        pex = ExitStack()
        actT = alloc(pex, "actT", [128, 8, TQ], BF16)
        NRING = 5
        ring = [alloc(pex, "ring%d" % i, [128, 8, 512], BF16) for i in range(NRING)]
        b1r = [alloc(pex, "b1r%d" % i, [1, 2048], BF16) for i in range(1)]
        t1 = [alloc(pex, "t1_%d" % i, [128, 512], F32) for i in range(1)]
        t2 = [alloc(pex, "t2_%d" % i, [128, 512], F32) for i in range(1)]
        t3 = [alloc(pex, "t3_%d" % i, [128, 512], F32) for i in range(1)]
        with ExitStack() as prt:
            wr = alloc(prt, "wr", [128, 8, 32], BF16); S.dma("pool", wr[:], w_router.rearrange("(c p) n -> p c n", p=128))
            br = alloc(prt, "br", [1, 32], BF16); S.dma("pool", br[:], b_router)
            b2_sb = alloc(prt, "b2_sb", [32, D], BF16); S.dma("pool", b2_sb[:], b2)
            for ti in range(NT):
                o = ps[0][:, 0:32]
                for kc in range(8):
                    S.mm(o, h1T[:, kc, ti * 128:(ti + 1) * 128], wr[:, kc, :], start=(kc == 0), stop=False)
                S.mm(o, ones_b[0:1, 0:128], br[0:1, :], start=False, stop=True)
                S.cp("act", lg[:], o)
                S.max8(m8[:], lg[:])
                S.ts("dve", ex[:], lg[:], m8[:, 0:1], None, ALU.subtract)
                S.act(ex[:], ex[:], AF.Exp)
                S.stt("dve", ex[:], lg[:], m8[:, 3:4], ex[:], ALU.is_ge, ALU.mult)
                S.red(sm2[:, 0:1], ex[:], ALU.add)
                S.recip(sm2[:, 1:2], sm2[:, 0:1])
                S.ts("dve", gates[:, ti, :], ex[:], sm2[:, 1:2], None, ALU.mult)
                S.cp("act", gb[:], gates[:, ti, :])
                S.tr(psT[0:32, 0:128], gb[:], ident_b[:])
                S.cp("act", gT[:, ti * 128:(ti + 1) * 128], psT[0:32, 0:128])
                for dh in range(2):
                    S.mm(ps[1 + dh][:], gT[:, ti * 128:(ti + 1) * 128], b2_sb[:, dh * 512:(dh + 1) * 512], start=True, stop=True)
                    S.cp("act", accm[:, ti, dh * 512:(dh + 1) * 512], ps[1 + dh][:])
        S.barrier()
        if "moe" in dbg:
            dg_ = dbg_tensor("gates", [128, NT * 32], F32)
            S.dma("sp", dg_, gates[:].rearrange("p a b -> p (a b)"))
        w1k = w1.rearrange("e (c p) n -> e p c n", p=128)
        w2k = w2.rearrange("e (c p) n -> e p c n", p=128)
        nexp = lim.get("nexp", 32)
        nr = 0
        nel = 0
        for e in range(nexp):
            brow = b1r[0]
            S.dma("pool", brow[:], b1[e:e + 1, :])
            for c in range(4):
                wc = ring[nr % NRING]
                nr += 1
                S.dma("pool", wc[:, :, 0:256], w1k[e, :, :, 256 * c:256 * c + 256])
                S.dma("pool", wc[:, :, 256:512], w1k[e, :, :, 1024 + 256 * c:1024 + 256 * c + 256])
                for tg in range(4):
                    rhs_t = slice(tg * 512, (tg + 1) * 512)
                    for fc in range(2):
                        pg_, pu_ = ps[2 * fc][:], ps[2 * fc + 1][:]
                        for (po_, off, boff) in ((pg_, fc * 128, 256 * c + fc * 128),
                                                 (pu_, 256 + fc * 128, 1024 + 256 * c + fc * 128)):
                            for kc in range(8):
                                S.mm(po_, wc[:, kc, off:off + 128], h1T[:, kc, rhs_t], start=(kc == 0), stop=False)
                            S.mm(po_, brow[0:1, boff:boff + 128], ones_b[0:1, 0:512], start=False, stop=True)
                        a1, a2, a3 = t1[0], t2[0], t3[0]
                        nel += 1
                        S.ts("dve", a1[:], pg_, 7.0, None, ALU.min)
                        S.act(a2[:], a1[:], AF.Sigmoid, scale=1.702)
                        S.tt("dve", a1[:], a1[:], a2[:], ALU.mult)
                        S.ts("dve", a3[:], pu_, -7.0, 7.0, ALU.max, ALU.min)
                        S.stt("dve", actT[:, 2 * c + fc, rhs_t], a3[:], 1.0, a1[:], ALU.add, ALU.mult)
            for dh in range(2):
                wc = ring[nr % NRING]
                nr += 1
                S.dma("pool", wc[:], w2k[e, :, :, dh * 512:(dh + 1) * 512])
                for ti in range(NT):
                    py = ps[4 + ti % 3][:]
                    for fc in range(8):
                        S.mm(py, actT[:, fc, ti * 128:(ti + 1) * 128], wc[:, fc, :], start=(fc == 0), stop=(fc == 7))
                    S.stt("dve", accm[:, ti, dh * 512:(dh + 1) * 512], py, gates[:, ti, e:e + 1],
                          accm[:, ti, dh * 512:(dh + 1) * 512], ALU.mult, ALU.add)
        pex.close()
        S.barrier()
        h1r = [alloc(pe_, "h1r%d" % i, [128, D], F32) for i in range(2)]
        lnt2 = alloc(pe_, "lnt2", [128, D], F32)
        yo = [alloc(pe_, "yo%d" % i, [128, D], F32) for i in range(2)]
        for ti in range(NT):
            hr = h1r[ti % 2]
            S.dma("sp", hr[:], scr_h1[ti * 128:(ti + 1) * 128, :])
            S.stt("dve", hr[:], hr[:], DN_ALPHA, accm[:, ti, :], ALU.mult, ALU.add)
            ln_tok(hr[:], g2_rep[:], b2_rep[:], yo[ti % 2][:], D, lnt2[:])
            S.dma("sp", y_out[ti * 128:(ti + 1) * 128, :], yo[ti % 2][:])
    S.finish()
    return nc, dbg_out, S


def _prep_inputs(inputs, stage=99):
    x = np.asarray(inputs["x"], np.float32)
    mem = np.asarray(inputs["mem"], np.float32)
    common = {}
    sq = {"ln0_g": (1, D), "ln0_b": (1, D), "w_in": (D, D_IN), "b_in": (1, D_IN), "kv_norm_g": (1, 128)}
    if stage >= 2:
        sq.update({"w_uk": (8, 128, 64), "w_uv": (8, 128, 64), "w_o_a": (512, D)})
    if stage >= 3:
        sq.update({"w_dw": (31, 512), "b_dw": (1, 512), "conv_ln_g": (1, 512), "conv_ln_b": (1, 512),
                   "w_pw2": (512, D), "b_pw2": (1, D), "w_mem_k": (D, 512), "w_mem_v": (D, 512),
                   "w_o_c": (512, D), "w_out": (D, D), "b_out": (1, D), "ln1_g": (1, D), "ln1_b": (1, D)})
    if stage >= 4:
        sq.update({"w_router": (D, 32), "b_router": (1, 32), "w1": (32, D, 2048), "b1": (32, 2048),
                   "w2": (32, D, D), "b2": (32, D), "ln2_g": (1, D), "ln2_b": (1, D)})
    for k, shp in sq.items():
        common[k] = np.ascontiguousarray(np.asarray(inputs[k], np.float32).reshape(shp))
    for k, v in _consts().items():
        common["c_" + k] = v
    in_maps = []
    for c in range(8):
        b, j = c // 4, c % 4
        m = dict(common)
        m["xk"] = np.ascontiguousarray(np.roll(x[b], -(j + 1) * TQ, axis=0))
        if stage >= 3:
            m["memb"] = np.ascontiguousarray(mem[b])
        cvv = np.zeros((128, 8), np.float32)
        for q in range(3):
            cvv[:, q] = 0.0 if q >= 3 - j else NEG
        cvv[:, 3] = 1.0 if j > 0 else 0.0
        m["cvec"] = cvv
        in_maps.append(m)
    return in_maps


def kernel(**inputs):
    in_maps = _prep_inputs(inputs)
    nc, _, _ = build()
    res = run_bass_kernel_spmd(nc, in_maps, core_ids=list(range(8)))
    out = np.zeros((2, SEQ, D), np.float32)
    for c in range(8):
        b, j = c // 4, c % 4
        out[b, j * TQ:(j + 1) * TQ] = np.asarray(res.results[c]["y"], np.float32)
    return out
```

```python
import numpy as np
import ml_dtypes
import concourse.bass as bass
import concourse.mybir as mybir
from concourse.bass_utils import run_bass_kernel_spmd
from contextlib import ExitStack

F32 = mybir.dt.float32
BF16 = mybir.dt.bfloat16
FP8 = mybir.dt.float8e5
ALU = mybir.AluOpType
AF = mybir.ActivationFunctionType
AX = mybir.AxisListType

D = 1024
SEQ = 8192
TQ = 2048
NT = 16
NKT = 64
D_IN = 5544
OFF_QA, OFF_CKV, OFF_QIDX, OFF_KIDX, OFF_WIDX, OFF_GLU, OFF_QC, OFF_GATE = 0, 512, 640, 896, 928, 936, 1960, 2472
LN_EPS = 1e-5
IDX_SCALE = 256 ** -0.5
DN_ALPHA = 2 ** 0.25
NEG = -1e30
MB = -28672.0
NITER = 12

_ES = {F32: 4, BF16: 2, FP8: 1, mybir.dt.int32: 4, mybir.dt.int16: 2, mybir.dt.uint16: 2, mybir.dt.uint32: 4}


def _esize(dt):
    return _ES.get(dt, 4)


class Sched:
    def __init__(self, nc, ndma=4):
        self.nc = nc
        self.e = dict(pe=nc.tensor, act=nc.scalar, dve=nc.vector, pool=nc.gpsimd, sp=nc.sync)
        self.sem = {k: nc.alloc_semaphore(name="s_" + k) for k in ("pe", "act", "dve", "pool")}
        self.cnt = {k: 0 for k in self.sem}
        self.ndma = ndma
        self.dsem = {}
        self.dcnt = {}
        for q in ("sp", "pool", "act"):
            self.dsem[q] = [nc.alloc_semaphore(name="d_%s%d" % (q, i)) for i in range(ndma)]
            self.dcnt[q] = 0
        self.seen = {k: {} for k in self.e}
        self.regs = {}
        self.nwait = 0
        self.nins = 0

    def region(self, ap):
        t = ap.tensor
        tn = type(t).__name__
        es = _esize(ap.dtype)
        dims = list(ap.ap)
        if "DRam" in tn:
            ext = sum((c - 1) * abs(s) for s, c in dims) + 1
            return ("D:" + t.name, 0, 1, ap.offset * es, (ap.offset + ext) * es)
        shape = list(t.shape)
        fsz = 1
        for s in shape[1:]:
            fsz *= s
        off = ap.offset
        p0 = off // fsz
        f0 = off % fsz
        pstep, pcnt = dims[0]
        if pstep == 0:
            pcnt = 1
        ext = sum((c - 1) * abs(s) for s, c in dims[1:]) + 1
        if "PSum" in tn:
            b0 = (f0 * es) // 2048 * 2048
            b1 = ((f0 + ext) * es + 2047) // 2048 * 2048
            return (t.name, 0, 128, b0, b1)
        return (t.name, p0, p0 + pcnt, f0 * es, (f0 + ext) * es)

    @staticmethod
    def _ov(a, b):
        return a[1] < b[2] and b[1] < a[2] and a[3] < b[4] and b[3] < a[4]

    def _deps_read(self, reg, deps):
        for rec in self.regs.get(reg[0], ()):
            if self._ov(rec[0], reg):
                for k, v in rec[1].items():
                    if deps.get(k, 0) < v:
                        deps[k] = v

    def _deps_write(self, reg, deps):
        for rec in self.regs.get(reg[0], ()):
            if self._ov(rec[0], reg):
                for dd in (rec[1], rec[2]):
                    for k, v in dd.items():
                        if deps.get(k, 0) < v:
                            deps[k] = v

    def _mark_read(self, reg, ev):
        k, v = ev
        for rec in self.regs.get(reg[0], ()):
            if self._ov(rec[0], reg):
                if rec[2].get(k, 0) < v:
                    rec[2][k] = v

    def _mark_write(self, reg, ev):
        lst = self.regs.setdefault(reg[0], [])
        keep = []
        for rec in lst:
            r = rec[0]
            if reg[1] <= r[1] and r[2] <= reg[2] and reg[3] <= r[3] and r[4] <= reg[4]:
                continue
            keep.append(rec)
        keep.append([reg, {ev[0]: ev[1]}, {}])
        if len(keep) > 40:
            p0 = min(r[0][1] for r in keep)
            p1 = max(r[0][2] for r in keep)
            f0 = min(r[0][3] for r in keep)
            f1 = max(r[0][4] for r in keep)
            w, rd = {}, {}
            for r in keep:
                for k, v in r[1].items():
                    w[k] = max(w.get(k, 0), v)
                for k, v in r[2].items():
                    rd[k] = max(rd.get(k, 0), v)
            keep = [[(reg[0], p0, p1, f0, f1), w, rd]]
        self.regs[reg[0]] = keep

    def _semof(self, key):
        if isinstance(key, tuple):
            return self.dsem[key[1]][key[2]]
        return self.sem[key]

    def _emit_waits(self, eng, deps, skip_same=None):
        for k, v in deps.items():
            if self.seen[eng].get(k, 0) >= v:
                continue
            self.e[eng].wait_ge(self._semof(k), v)
            self.seen[eng][k] = v
            self.nwait += 1

    def op(self, eng, fn, reads=(), writes=()):
        deps = {}
        rregs = [self.region(a) for a in reads if a is not None and hasattr(a, "tensor")]
        wregs = [self.region(a) for a in writes if a is not None and hasattr(a, "tensor")]
        for r in rregs:
            self._deps_read(r, deps)
        wdeps = {}
        for w in wregs:
            self._deps_write(w, wdeps)
        for k, v in wdeps.items():
            if k == eng and eng == "pe":
                continue
            if deps.get(k, 0) < v:
                deps[k] = v
        self._emit_waits(eng, deps)
        ins = fn()
        self.cnt[eng] += 1
        ev = (eng, self.cnt[eng])
        ins.then_inc(self.sem[eng], 1)
        self.nins += 1
        for r in rregs:
            self._mark_read(r, ev)
        for w in wregs:
            self._mark_write(w, ev)
        return ins

    def dma(self, q, out, in_, **kw):
        deps = {}
        rr = self.region(in_)
        wr = self.region(out)
        track_r = not rr[0].startswith("D:") or rr[0] in self.regs or rr[0].startswith("D:scr")
        track_w = True
        if track_r:
            self._deps_read(rr, deps)
        self._deps_write(wr, deps)
        n = self.dcnt[q]
        slot = n % self.ndma
        key = ("dma", q, slot)
        prev = 16 * (n // self.ndma)
        if prev > 0:
            deps[key] = max(deps.get(key, 0), prev)
        self._emit_waits(q, deps)
        ins = self.e[q].dma_start(out=out, in_=in_, **kw)
        val = 16 * (n // self.ndma + 1)
        ins.then_inc(self.dsem[q][slot], 16)
        self.dcnt[q] = n + 1
        ev = (key, val)
        if track_r:
            self._mark_read(rr, ev)
        if track_w:
            self._mark_write(wr, ev)
        self.nins += 1
        return ins

    def barrier(self):
        deps = {}
        for q in ("sp", "pool", "act"):
            n = self.dcnt[q]
            for slot in range(self.ndma):
                cntslot = (n - slot + self.ndma - 1) // self.ndma if n > slot else 0
                if cntslot > 0:
                    deps[("dma", q, slot)] = 16 * cntslot
        for k in self.sem:
            if self.cnt[k] > 0:
                deps[k] = self.cnt[k]
        for eng in self.e:
            self._emit_waits(eng, dict(deps))

    def finish(self):
        deps = {}
        for q in ("sp", "pool", "act"):
            n = self.dcnt[q]
            for slot in range(self.ndma):
                cntslot = (n - slot + self.ndma - 1) // self.ndma if n > slot else 0
                if cntslot > 0:
                    deps[("dma", q, slot)] = 16 * cntslot
        for k in self.sem:
            if self.cnt[k] > 0:
                deps[k] = self.cnt[k]
        self._emit_waits("sp", deps)

    def mm(self, out, lhsT, rhs, start=True, stop=True):
        return self.op("pe", lambda: self.nc.tensor.matmul(out, lhsT, rhs, start=start, stop=stop,
                                                          skip_group_check=True),
                       reads=(lhsT, rhs), writes=(out,))

    def tr(self, out, in_, ident):
        return self.op("pe", lambda: self.nc.tensor.transpose(out, in_, ident), reads=(in_, ident), writes=(out,))

    def act(self, out, in_, func, bias=None, scale=None, accum_out=None):
        kw = {}
        if bias is not None:
            kw["bias"] = bias
        if scale is not None:
            kw["scale"] = scale
        if accum_out is not None:
            kw["accum_out"] = accum_out
        return self.op("act", lambda: self.nc.scalar.activation(out, in_, func, **kw),
                       reads=(in_, bias, scale), writes=(out, accum_out))

    def ts(self, eng, out, in0, s1, s2, op0, op1=None, accum_out=None):
        e = self.e[eng]
        kw = {}
        if op1 is not None:
            kw["op1"] = op1
        if accum_out is not None:
            kw["accum_out"] = accum_out
        if out.dtype == FP8:
            kw["saturate"] = False
        return self.op(eng, lambda: e.tensor_scalar(out, in0, s1, s2, op0, **kw),
                       reads=(in0, s1, s2), writes=(out, accum_out))

    def tt(self, eng, out, in0, in1, op):
        e = self.e[eng]
        return self.op(eng, lambda: e.tensor_tensor(out, in0, in1, op), reads=(in0, in1), writes=(out,))

    def stt(self, eng, out, in0, scalar, in1, op0, op1):
        e = self.e[eng]
        return self.op(eng, lambda: e.scalar_tensor_tensor(out, in0, scalar, in1, op0, op1),
                       reads=(in0, scalar, in1), writes=(out,))

    def cp(self, eng, out, in_):
        e = self.e[eng]
        if eng == "act":
            return self.op(eng, lambda: e.copy(out, in_), reads=(in_,), writes=(out,))
        return self.op(eng, lambda: e.tensor_copy(out, in_), reads=(in_,), writes=(out,))

    def memset(self, eng, out, val):
        e = self.e[eng]
        return self.op(eng, lambda: e.memset(out, val), writes=(out,))

    def recip(self, out, in_):
        return self.op("dve", lambda: self.nc.vector.reciprocal(out, in_), reads=(in_,), writes=(out,))

    def red(self, out, in_, op, axis=AX.X):
        return self.op("dve", lambda: self.nc.vector.tensor_reduce(out, in_, axis, op), reads=(in_,), writes=(out,))

    def max8(self, out, in_):
        return self.op("dve", lambda: self.nc.vector.max(out, in_), reads=(in_,), writes=(out,))


def _consts():
    slopes = np.exp2(-(np.arange(1, 9, dtype=np.float64)))
    ident = np.eye(128, dtype=np.float32)
    E = np.tile(ident, (1, 8)).astype(np.float32)
    sl = np.arange(128)[:, None]
    tl = np.arange(128)[None, :]
    cd = np.zeros((128, 8, 128), np.float32)
    for h in range(8):
        cd[:, h, :] = 8.0 * slopes[h] * (tl - np.abs(tl - sl))
    cd = cd.reshape(128, 1024)
    spos = np.arange(SEQ)
    lpos = np.zeros((48, SEQ), np.float32)
    lpos[0:16] = 1.0
    lpos[32:40] = (spos & 127)[None, :]
    lpos[40:48] = (spos >> 7)[None, :]
    ldiag = np.zeros((48, NT, 128), np.float32)
    ldiag[0:16] = 1.0
    for i in range(NT):
        ldiag[40:48, i, :] = 48 + i
    rb = np.zeros((48, 8, 128), np.float32)
    bd = np.zeros((16, 8, 128), np.float32)
    for h in range(8):
        rb[32 + h, h, :] = 8.0 * slopes[h]
        rb[40 + h, h, :] = 8.0 * 128.0 * slopes[h]
        bd[h, h, :] = 1.0
        bd[8 + h, h, :] = 1.0
    rb = rb.reshape(48, 1024)
    bd = bd.reshape(16, 1024)
    dg = np.zeros((128, 128), np.float32)
    dg[:64, 64:] = NEG
    slope8 = np.tile((8.0 * slopes).astype(np.float32)[None, :], (128, 1))
    jpos = np.tile(((np.arange(128) + 1) * 64).astype(np.float32)[None, :], (128, 1))
    tlc = np.arange(128, dtype=np.float32)[:, None].copy()
    half = np.tile((0.5 ** (np.arange(NITER) + 1)).astype(np.float32)[None, :], (128, 1))
    return dict(ident=ident, E=E, cd=cd, lpos=lpos, ldiag=ldiag.reshape(48, NT * 128), rb=rb, bd=bd, dg=dg,
                slope8=slope8, jpos=jpos, tl=tlc, half=half)


def build(stage=99, dbg=(), lim=None):
    lim = lim or {}
    nc = bass.Bass("TRN2", target_bir_lowering=False)
    S = Sched(nc)
    dbg_out = {}

    def din(name, shape, dt=F32):
        return nc.dram_tensor(name, list(shape), dt, kind="ExternalInput").ap()

    xk = din("xk", [SEQ, D])
    cvec = din("cvec", [128, 8])
    ln0_g = din("ln0_g", [1, D]); ln0_b = din("ln0_b", [1, D])
    w_in = din("w_in", [D, D_IN]); b_in = din("b_in", [1, D_IN])
    kv_norm_g = din("kv_norm_g", [1, 128])
    C = {k: din("c_" + k, v.shape) for k, v in _consts().items()}
    if stage >= 2:
        w_uk = din("w_uk", [8, 128, 64]); w_uv = din("w_uv", [8, 128, 64])
        w_o_a = din("w_o_a", [512, D])
    if stage >= 3:
        memb = din("memb", [256, D])
        w_dw = din("w_dw", [31, 512]); b_dw = din("b_dw", [1, 512])
        conv_ln_g = din("conv_ln_g", [1, 512]); conv_ln_b = din("conv_ln_b", [1, 512])
        w_pw2 = din("w_pw2", [512, D]); b_pw2 = din("b_pw2", [1, D])
        w_mem_k = din("w_mem_k", [D, 512]); w_mem_v = din("w_mem_v", [D, 512])
        w_o_c = din("w_o_c", [512, D])
        w_out = din("w_out", [D, D]); b_out = din("b_out", [1, D])
        ln1_g = din("ln1_g", [1, D]); ln1_b = din("ln1_b", [1, D])
    if stage >= 4:
        w_router = din("w_router", [D, 32]); b_router = din("b_router", [1, 32])
        w1 = din("w1", [32, D, 2048]); b1 = din("b1", [32, 2048])
        w2 = din("w2", [32, D, D]); b2 = din("b2", [32, D])
        ln2_g = din("ln2_g", [1, D]); ln2_b = din("ln2_b", [1, D])
    y_out = nc.dram_tensor("y", [TQ, D], F32, kind="ExternalOutput").ap()

    def dbg_tensor(name, shape, dt=F32):
        t = nc.dram_tensor("dbg_" + name, list(shape), dt, kind="ExternalOutput").ap()
        dbg_out[name] = t
        return t

    scr_hT = nc.dram_tensor("scr_hT", [128, 8, 128 + TQ], BF16, kind="Internal").ap()
    scr_ya = nc.dram_tensor("scr_ya", [128, 8, TQ], BF16, kind="Internal").ap()
    scr_h1 = nc.dram_tensor("scr_h1", [TQ, D], F32, kind="Internal").ap()
    scr_h = nc.dram_tensor("scr_h", [TQ, D], F32, kind="Internal").ap()
    scr_h1T = nc.dram_tensor("scr_h1T", [128, 8, TQ], BF16, kind="Internal").ap()

    def alloc(stack, name, shape, dt):
        return stack.enter_context(nc.sbuf_tensor(name, list(shape), dt))

    top = ExitStack()
    ident_b = alloc(top, "ident_b", [128, 128], BF16)
    ident_f = alloc(top, "ident_f", [128, 128], F32)
    ones_b = alloc(top, "ones_b", [128, 512], BF16)
    cv = alloc(top, "cv", [128, 8], F32)
    eps_t = alloc(top, "eps_t", [128, 1], F32)
    st = alloc(top, "st", [128, 16], F32)
    junk = alloc(top, "junk", [128, D], F32)
    S.dma("pool", ident_b[:], C["ident"])
    S.dma("sp", ident_f[:], C["ident"])
    S.dma("sp", cv[:], cvec)
    S.memset("dve", ones_b[:], 1.0)
    S.memset("dve", eps_t[:], LN_EPS)

    ps = [nc.alloc_psum_tensor("ps%d" % i, [128, 512], F32) for i in range(7)]
    psT = nc.alloc_psum_tensor("psT", [128, 1024], BF16)


    def ln_tok(x_ap, g_rep, b_rep, out_ap, n, tmp_ap):
        S.act(junk[:, 0:n], x_ap, AF.Identity, accum_out=st[:, 0:1])
        S.act(junk[:, 0:n], x_ap, AF.Square, accum_out=st[:, 1:2])
        S.ts("dve", st[:, 2:3], st[:, 0:1], 1.0 / n, None, ALU.mult)
        S.tt("dve", st[:, 3:4], st[:, 2:3], st[:, 2:3], ALU.mult)
        S.stt("dve", st[:, 4:5], st[:, 1:2], 1.0 / n, st[:, 3:4], ALU.mult, ALU.subtract)
        S.act(st[:, 5:6], st[:, 4:5], AF.Sqrt, bias=eps_t[:, 0:1])
        S.recip(st[:, 6:7], st[:, 5:6])
        S.stt("dve", st[:, 7:8], st[:, 2:3], -1.0, st[:, 6:7], ALU.mult, ALU.mult)
        S.act(tmp_ap, x_ap, AF.Identity, bias=st[:, 7:8], scale=st[:, 6:7])
        S.tt("dve", tmp_ap, tmp_ap, g_rep, ALU.mult)
        S.tt("dve", out_ap, tmp_ap, b_rep, ALU.add)

    w_in_k = w_in.rearrange("(kc p) n -> p kc n", p=128)

    def load_col(dst, row_ap):
        S.dma("sp", dst, row_ap.rearrange("(c p) -> p c", p=128), allow_slow_non_contiguous=True)

    keys = ExitStack()
    ckvT = alloc(keys, "ckvT", [128, SEQ], BF16)
    ckv1 = alloc(keys, "ckv1", [128, NKT, 129], BF16)
    kidxT = alloc(keys, "kidxT", [128, SEQ], BF16)
    S.memset("pool", ckv1[:, :, 128:129], 1.0)
    nkt = lim.get("nkt", NKT)
    with ExitStack() as pk:
        g0_rep = alloc(pk, "g0_rep", [128, D], F32)
        b0_rep = alloc(pk, "b0_rep", [128, D], F32)
        S.dma("sp", g0_rep[:], ln0_g.partition_broadcast(128))
        S.dma("sp", b0_rep[:], ln0_b.partition_broadcast(128))
        wkv = alloc(pk, "wkv", [128, 8, 256], BF16)
        S.dma("pool", wkv[:, :, 0:128], w_in_k[:, :, OFF_CKV:OFF_CKV + 128])
        for r in range(4):
            S.dma("pool", wkv[:, :, 128 + 32 * r:160 + 32 * r], w_in_k[:, :, OFF_KIDX:OFF_KIDX + 32])
        bkv = alloc(pk, "bkv", [1, 256], BF16)
        S.dma("pool", bkv[:, 0:128], b_in[:, OFF_CKV:OFF_CKV + 128])
        for r in range(4):
            S.dma("pool", bkv[:, 128 + 32 * r:160 + 32 * r], b_in[:, OFF_KIDX:OFF_KIDX + 32])
        gkv_rep = alloc(pk, "gkv_rep", [128, 128], F32)
        S.dma("sp", gkv_rep[:], kv_norm_g.partition_broadcast(128))
        xbuf = [alloc(pk, "xbuf%d" % i, [128, D], F32) for i in range(2)]
        xn = alloc(pk, "xn", [128, D], F32)
        hb = alloc(pk, "hb", [128, D], BF16)
        hf = alloc(pk, "hf", [128, D], F32)
        hTt = [alloc(pk, "hTt%d" % i, [128, 8, 128], BF16) for i in range(2)]
        kx = alloc(pk, "kx", [128, 128], BF16)
        for kt in range(nkt):
            xt = xbuf[kt % 2]
            S.dma("sp", xt[:], xk[kt * 128:(kt + 1) * 128, :])
            if kt >= 48:
                ln_tok(xt[:], g0_rep[:], b0_rep[:], hf[:], D, xn[:])
                S.dma("sp", scr_h[(kt - 48) * 128:(kt - 47) * 128, :], hf[:])
                S.cp("act", hb[:], hf[:])
            else:
                ln_tok(xt[:], g0_rep[:], b0_rep[:], hb[:], D, xn[:])
            for kc in range(8):
                S.tr(psT[:, kc * 128:(kc + 1) * 128], hb[:, kc * 128:(kc + 1) * 128], ident_b[:])
            ht = hTt[kt % 2]
            S.cp("act", ht[:].rearrange("p a b -> p (a b)"), psT[:])
            if kt >= 47:
                S.dma("sp", scr_hT[:, :, (kt - 47) * 128:(kt - 46) * 128], ht[:])
            pk_ = ps[kt % 2]
            for kc in range(8):
                S.mm(pk_[:, 0:256], ht[:, kc, :], wkv[:, kc, :], start=(kc == 0), stop=False)
            S.mm(pk_[:, 0:256], ones_b[0:1, 0:128], bkv[0:1, :], start=False, stop=True)
            S.act(junk[:, 0:128], pk_[:, 0:128], AF.Square, accum_out=st[:, 8:9])
            S.ts("dve", st[:, 9:10], st[:, 8:9], 1.0 / 128, None, ALU.mult)
            S.act(st[:, 10:11], st[:, 9:10], AF.Sqrt, bias=eps_t[:, 0:1])
            S.recip(st[:, 11:12], st[:, 10:11])
            S.stt("dve", ckv1[:, kt, 0:128], pk_[:, 0:128], st[:, 11:12], gkv_rep[:], ALU.mult, ALU.mult)
            S.cp("act", kx[:], pk_[:, 128:256])
            S.tr(psT[:, 0:128], ckv1[:, kt, 0:128], ident_b[:])
            S.tr(psT[:, 128:256], kx[:], ident_b[:])
            S.cp("act", ckvT[:, kt * 128:(kt + 1) * 128], psT[:, 0:128])
            S.cp("act", kidxT[:, kt * 128:(kt + 1) * 128], psT[:, 128:256])

    S.barrier()
    if "keys" in dbg:
        d1 = dbg_tensor("ckvT", [128, SEQ], BF16)
        d2 = dbg_tensor("kidxT", [128, SEQ], BF16)
        S.dma("sp", d1[:, 0:nkt * 128], ckvT[:, 0:nkt * 128])
        S.dma("sp", d2[:, 0:nkt * 128], kidxT[:, 0:nkt * 128])

    if stage <= 1:
        S.finish()
        return nc, dbg_out, S

    class _Stop(Exception):
        pass

    def ckpt(k):
        if lim.get("a_stop", 99) <= k:
            raise _Stop()

    try:
      with ExitStack() as pa:
          E_b = alloc(pa, "E_b", [128, 1024], BF16); S.dma("pool", E_b[:], C["E"])
          cd_b = alloc(pa, "cd_b", [128, 1024], BF16); S.dma("pool", cd_b[:], C["cd"])
          lpos_b = alloc(pa, "lpos_b", [48, SEQ], BF16); S.dma("pool", lpos_b[:], C["lpos"])
          ldiag_b = alloc(pa, "ldiag_b", [48, NT * 128], BF16); S.dma("pool", ldiag_b[:], C["ldiag"])
          Rb = [alloc(pa, "Rb%d" % k, [48, 1024], BF16) for k in range(2)]
          for k in range(2):
              S.dma("pool", Rb[k][:], C["rb"])
          bd_b = alloc(pa, "bd_b", [16, 1024], BF16); S.dma("pool", bd_b[:], C["bd"])
          dg = alloc(pa, "dg", [128, 128], F32); S.dma("sp", dg[:], C["dg"])
          slope8 = alloc(pa, "slope8", [128, 8], F32); S.dma("sp", slope8[:], C["slope8"])
          jpos = alloc(pa, "jpos", [128, 128], F32); S.dma("sp", jpos[:], C["jpos"])
          tlc = alloc(pa, "tlc", [128, 1], F32); S.dma("sp", tlc[:], C["tl"])
          half = alloc(pa, "half", [128, NITER], F32); S.dma("sp", half[:], C["half"])
          wq = alloc(pa, "wq", [128, 8, 776], BF16)
          bq = alloc(pa, "bq", [1, 776], BF16)
          for (dst0, src0, n) in ((0, OFF_QA, 512), (512, OFF_QIDX, 256), (768, OFF_WIDX, 8)):
              S.dma("pool", wq[:, :, dst0:dst0 + n], w_in_k[:, :, src0:src0 + n])
              S.dma("pool", bq[:, dst0:dst0 + n], b_in[:, src0:src0 + n])
          wuk_b = alloc(pa, "wuk_b", [128, 8, 64], BF16)
          S.dma("pool", wuk_b[:], w_uk.rearrange("h r d -> r h d"))
          wuv = alloc(pa, "wuv", [128, 8, 64], BF16)
          S.dma("pool", wuv[:], w_uv.rearrange("h r d -> r h d"))
          woa = alloc(pa, "woa", [64, 8, D], BF16)
          S.dma("pool", woa[:], w_o_a.rearrange("(h d) n -> d h n", d=64))
          wukT = alloc(pa, "wukT", [64, 8, 128], BF16)
          for hh in range(8):
              S.tr(psT[0:64, hh * 128:(hh + 1) * 128], wuk_b[:, hh, :], ident_b[:])
          S.cp("act", wukT[:].rearrange("p a b -> p (a b)"), psT[0:64, :])

          ckpt(1)
          acc = alloc(pa, "acc", [128, SEQ], F32)
          maskb = [alloc(pa, "maskb%d" % k, [128, SEQ], BF16) for k in range(2)]
          hTi = [alloc(pa, "hTi%d" % k, [128, 8, 128], BF16) for k in range(2)]
          qa_sb = alloc(pa, "qa_sb", [64, 8, 128], BF16)
          qlat = [alloc(pa, "qlat%d" % k, [128, 1024], BF16) for k in range(2)]
          qi = alloc(pa, "qi", [32, 8, 128], BF16)
          wabs = alloc(pa, "wabs", [128, 8], F32)
          sgn = alloc(pa, "sgn", [128, 8], F32)
          rt = [alloc(pa, "rt%d" % i, [128, 512], F32) for i in range(2)]
          PT = [alloc(pa, "PT%d" % i, [128, 512], BF16) for i in range(2)]
          sm = alloc(pa, "sm", [128, 16], F32)
          steps = alloc(pa, "steps", [128, NITER], F32)
          r1 = alloc(pa, "r1", [128, 128], F32)
          v8 = alloc(pa, "v8", [128, 8], F32)
          vhf = alloc(pa, "vhf", [128, 8], F32)
          vhl = alloc(pa, "vhl", [128, 16], BF16)
          vT = alloc(pa, "vT", [16, 128], BF16)
          rden = alloc(pa, "rden", [128, 8], F32)
          olat = alloc(pa, "olat", [128, 8, 128], BF16)
          olatT = alloc(pa, "olatT", [128, 1024], BF16)
          z_sb = qa_sb
          yat = olat
          Amax, Wd, lo, mid, cnt, tmp1, m1, ddv = [sm[:, k:k + 1] for k in range(8)]

          def po(hh):
              bank = ps[2 + hh // 3]
              k = hh % 3
              return bank[:, k * 129:(k + 1) * 129]

          ntl = lim.get("nt", NT)

          def qproj(i):
              p = i % 2
              hT_ = hTi[p]
              S.dma("sp", hT_[:], scr_hT[:, :, 128 + i * 128:128 + (i + 1) * 128])
              for hh in range(8):
                  o = ps[5 + hh // 4][0:64, (hh % 4) * 128:(hh % 4 + 1) * 128]
                  for kc in range(8):
                      S.mm(o, wq[:, kc, hh * 64:(hh + 1) * 64], hT_[:, kc, :], start=(kc == 0), stop=False)
                  S.mm(o, bq[0:1, hh * 64:(hh + 1) * 64], ones_b[0:1, 0:128], start=False, stop=True)
              S.cp("act", qa_sb[:, 0:4, :].rearrange("p a b -> p (a b)"), ps[5][0:64, :])
              S.cp("act", qa_sb[:, 4:8, :].rearrange("p a b -> p (a b)"), ps[6][0:64, :])
              for hh in range(8):
                  o = ps[5 + hh // 4][:, (hh % 4) * 128:(hh % 4 + 1) * 128]
                  S.mm(o, wukT[:, hh, :], qa_sb[:, hh, :], start=True, stop=True)
              S.cp("act", qlat[p][:, 0:512], ps[5][:])
              S.cp("act", qlat[p][:, 512:1024], ps[6][:])
              for hh in range(8):
                  o = ps[5 + hh // 4][0:32, (hh % 4) * 128:(hh % 4 + 1) * 128]
                  c0 = 512 + hh * 32
                  for kc in range(8):
                      S.mm(o, wq[:, kc, c0:c0 + 32], hT_[:, kc, :], start=(kc == 0), stop=False)
                  S.mm(o, bq[0:1, c0:c0 + 32], ones_b[0:1, 0:128], start=False, stop=True)
              S.cp("act", qi[:, 0:4, :].rearrange("p a b -> p (a b)"), ps[5][0:32, :])
              S.cp("act", qi[:, 4:8, :].rearrange("p a b -> p (a b)"), ps[6][0:32, :])
              for kc in range(8):
                  S.mm(ps[6][:, 0:8], hT_[:, kc, :], wq[:, kc, 768:776], start=(kc == 0), stop=False)
              S.mm(ps[6][:, 0:8], ones_b[0:1, 0:128], bq[0:1, 768:776], start=False, stop=True)
              S.act(wabs[:], ps[6][:, 0:8], AF.Abs, scale=IDX_SCALE)
              S.ts("dve", sgn[:], ps[6][:, 0:8], 0.0, 2.0, ALU.is_ge, ALU.mult)
              S.ts("dve", sgn[:], sgn[:], -1.0, None, ALU.add)

          def scores(i):
              it = 48 + i
              L = (it + 1) * 128
              nch = (L + 511) // 512
              n = 0
              for hh in range(8):
                  for sc in range(nch):
                      c0 = sc * 512
                      c1 = min(L, c0 + 512)
                      w = c1 - c0
                      pss = ps[5 + n % 2]
                      rtt = rt[n % 2]
                      n += 1
                      S.mm(pss[:, 0:w], qi[:, hh, :], kidxT[0:32, c0:c1], start=True, stop=True)
                      S.act(rtt[:, 0:w], pss[:, 0:w], AF.Relu, scale=wabs[:, hh:hh + 1])
                      if hh == 0:
                          S.ts("dve", acc[:, c0:c1], rtt[:, 0:w], sgn[:, 0:1], None, ALU.mult)
                      else:
                          S.stt("dve", acc[:, c0:c1], rtt[:, 0:w], sgn[:, hh:hh + 1], acc[:, c0:c1],
                                ALU.mult, ALU.add)

          def bisect(i):
              p = i % 2
              it = 48 + i
              L = (it + 1) * 128
              base = float(6144 + 128 * i)
              mk = maskb[p]
              S.red(Amax, acc[:, 0:L], ALU.max)
              S.red(tmp1, acc[:, 0:L], ALU.min)
              S.stt("dve", Amax, tmp1, -1.0, Amax, ALU.mult, ALU.max)
              for q in range(3):
                  S.ts("dve", acc[:, q * TQ:(q + 1) * TQ], acc[:, q * TQ:(q + 1) * TQ], cv[:, q:q + 1], None, ALU.add)
              S.tt("dve", acc[:, it * 128:L], acc[:, it * 128:L], dg[:], ALU.add)
              S.ts("dve", Wd, Amax, 2.002, 2e-6, ALU.mult, ALU.add)
              S.ts("dve", lo, Wd, -0.5, None, ALU.mult)
              S.ts("dve", steps[:], half[:], Wd, None, ALU.mult)
              for k in range(NITER):
                  S.tt("dve", mid, lo, steps[:, k:k + 1], ALU.add)
                  S.ts("dve", mk[:, 0:L], acc[:, 0:L], mid, None, ALU.is_ge, op1=ALU.add, accum_out=cnt)
                  S.stt("dve", tmp1, cnt, 255.5, steps[:, k:k + 1], ALU.is_ge, ALU.mult)
                  S.tt("dve", lo, lo, tmp1, ALU.add)
              S.ts("dve", mk[:, 0:L], acc[:, 0:L], lo, MB, ALU.is_lt, ALU.mult)
              nh = L // 64
              S.red(r1[:, 0:nh], mk[:, 0:L].rearrange("p (a b) -> p a b", b=64), ALU.max)
              S.tt("dve", r1[:, 0:nh], r1[:, 0:nh], jpos[:, 0:nh], ALU.add)
              S.red(m1, r1[:, 0:nh], ALU.max)
              S.ts("dve", ddv, m1, -1.0, tlc[:, 0:1], ALU.mult, ALU.add)
              S.ts("dve", ddv, ddv, base, 0.0, ALU.add, ALU.max)
              S.ts("dve", ddv, ddv, tlc[:, 0:1], base, ALU.subtract, ALU.subtract)
              S.ts("dve", v8[:], slope8[:], ddv, None, ALU.mult)
              S.cp("dve", vhl[:, 0:8], v8[:])
              S.cp("dve", vhf[:], vhl[:, 0:8])
              S.tt("dve", vhl[:, 8:16], v8[:], vhf[:], ALU.subtract)
              S.tr(psT[0:16, 0:128], vhl[:], ident_b[:])
              S.cp("act", vT[:], psT[0:16, 0:128])
              S.tt("dve", Rb[p][0:16, :].rearrange("p (h t) -> p h t", h=8),
                   vT[:].unsqueeze(1).to_broadcast([16, 8, 128]),
                   bd_b[:].rearrange("p (h t) -> p h t", h=8), ALU.mult)
              if "dsa" in dbg and i == 0:
                  dd1 = dbg_tensor("maskb", [128, SEQ], F32)
                  S.dma("pool", dd1[:, 0:L], mk[:, 0:L])
                  dd2 = dbg_tensor("sm", [128, 16], F32)
                  S.dma("sp", dd2[:, 0:8], sm[:, 0:8])
                  dd3 = dbg_tensor("acc", [128, SEQ], F32)
                  S.dma("sp", dd3[:, 0:L], acc[:, 0:L])
                  dd4 = dbg_tensor("qlat", [128, 1024], BF16)
                  S.dma("sp", dd4, qlat[p][:])

          def attn(i):
              p = i % 2
              it = 48 + i
              mk = maskb[p]
              first_in_bank = {0: True, 3: True, 6: True}
              chunks = [(j, c) for j in range(it + 1) for c in range(2)]

              def logits(n):
                  j, c = chunks[n]
                  pl = ps[n % 2]
                  ptt = PT[n % 2]
                  cs = slice(c * 512, (c + 1) * 512)
                  S.mm(pl[:], ckvT[:, j * 128:(j + 1) * 128], qlat[p][:, cs], start=True, stop=False)
                  if j < it:
                      S.mm(pl[:], lpos_b[0:48, j * 128:(j + 1) * 128], Rb[p][0:48, cs], start=False, stop=False)
                  else:
                      S.mm(pl[:], ldiag_b[0:48, i * 128:(i + 1) * 128], Rb[p][0:48, cs], start=False, stop=False)
                      S.mm(pl[:], ident_b[:], cd_b[:, cs], start=False, stop=False)
                  S.mm(pl[:], mk[:, j * 128:(j + 1) * 128], E_b[:, cs], start=False, stop=True)
                  S.act(ptt[:], pl[:], AF.Exp, scale=0.125)

              def pv(n):
                  j, c = chunks[n]
                  ptt = PT[n % 2]
                  for h4 in range(4):
                      hh = 4 * c + h4
                      S.mm(po(hh), ptt[:, h4 * 128:(h4 + 1) * 128], ckv1[:, j, :],
                           start=(j == 0 and hh in first_in_bank), stop=(j == it))

              logits(0)
              for n in range(len(chunks)):
                  if n + 1 < len(chunks):
                      logits(n + 1)
                  pv(n)

          def fin(i):
              for hh in range(8):
                  S.recip(rden[:, hh:hh + 1], po(hh)[:, 128:129])
              for hh in range(8):
                  S.act(olat[:, hh, :], po(hh)[:, 0:128], AF.Identity, scale=rden[:, hh:hh + 1])
              for hh in range(8):
                  S.tr(psT[:, hh * 128:(hh + 1) * 128], olat[:, hh, :], ident_b[:])
              S.cp("act", olatT[:], psT[:])
              for hh in range(8):
                  o = ps[5 + hh // 4][0:64, (hh % 4) * 128:(hh % 4 + 1) * 128]
                  S.mm(o, wuv[:, hh, :], olatT[:, hh * 128:(hh + 1) * 128], start=True, stop=True)
              S.cp("act", z_sb[:, 0:4, :].rearrange("p a b -> p (a b)"), ps[5][0:64, :])
              S.cp("act", z_sb[:, 4:8, :].rearrange("p a b -> p (a b)"), ps[6][0:64, :])
              for dc in range(8):
                  o = ps[5 + dc // 4][:, (dc % 4) * 128:(dc % 4 + 1) * 128]
                  for hh in range(8):
                      S.mm(o, woa[:, hh, dc * 128:(dc + 1) * 128], z_sb[:, hh, :], start=(hh == 0), stop=(hh == 7))
              S.cp("act", yat[:, 0:4, :].rearrange("p a b -> p (a b)"), ps[5][:])
              S.cp("act", yat[:, 4:8, :].rearrange("p a b -> p (a b)"), ps[6][:])
              S.dma("sp", scr_ya[:, :, i * 128:(i + 1) * 128], yat[:])

          qproj(0)
          scores(0)
          bisect(0)
          for i in range(ntl):
              if i + 1 < ntl:
                  qproj(i + 1)
                  scores(i + 1)
              if lim.get("bar1", False):
                  S.barrier()
              attn(i)
              if i + 1 < ntl:
                  bisect(i + 1)
              S.barrier()
              fin(i)
          if "dsa" in dbg:
              dd5 = dbg_tensor("ya", [128, 8, TQ], BF16)
              S.dma("sp", dd5[:, :, 0:ntl * 128], scr_ya[:, :, 0:ntl * 128])
    except _Stop:
        S.finish()
        return nc, dbg_out, S
    keys.close()
    S.barrier()

    if stage <= 2:
        S.finish()
        return nc, dbg_out, S

    TG = 256
    C_SCALE = 128 ** -0.5
    with ExitStack() as pm:
        g1_rep = alloc(pm, "g1_rep", [128, D], F32); S.dma("sp", g1_rep[:], ln1_g.partition_broadcast(128))
        b1_rep = alloc(pm, "b1_rep", [128, D], F32); S.dma("sp", b1_rep[:], ln1_b.partition_broadcast(128))
        onesf = alloc(pm, "onesf", [128, 128], F32); S.memset("dve", onesf[:], 1.0 / 512)
        wglu = alloc(pm, "wglu", [128, 8, 1024], BF16); S.dma("pool", wglu[:], w_in_k[:, :, OFF_GLU:OFF_GLU + 1024])
        wqc = alloc(pm, "wqc", [128, 8, 512], BF16); S.dma("pool", wqc[:], w_in_k[:, :, OFF_QC:OFF_QC + 512])
        wgate = alloc(pm, "wgate", [128, 8, 3072], BF16)
        for k in range(3):
            S.dma("pool", wgate[:, :, k * 1024:(k + 1) * 1024], w_in_k[:, :, OFF_GATE + k * 1024:OFF_GATE + (k + 1) * 1024])
        wpw2 = alloc(pm, "wpw2", [128, 4, D], BF16); S.dma("pool", wpw2[:], w_pw2.rearrange("(c p) n -> p c n", p=128))
        woc = alloc(pm, "woc", [128, 4, D], BF16); S.dma("pool", woc[:], w_o_c.rearrange("(c p) n -> p c n", p=128))
        wout = alloc(pm, "wout", [128, 8, D], BF16); S.dma("pool", wout[:], w_out.rearrange("(c p) n -> p c n", p=128))
        bout = alloc(pm, "bout", [1, D], BF16); S.dma("pool", bout[:], b_out)
        bglu = alloc(pm, "bglu", [128, 8], F32); load_col(bglu[:], b_in[0, OFF_GLU:OFF_GLU + 1024])
        bqc = alloc(pm, "bqc", [128, 4], F32); load_col(bqc[:], b_in[0, OFF_QC:OFF_QC + 512])
        bgate = alloc(pm, "bgate", [128, 24], F32); load_col(bgate[:], b_in[0, OFF_GATE:OFF_GATE + 3072])
        bpw2 = alloc(pm, "bpw2", [128, 8], F32); load_col(bpw2[:], b_pw2[0, :])
        bdw = alloc(pm, "bdw", [128, 4], F32); load_col(bdw[:], b_dw[0, :])
        cg = alloc(pm, "cg", [128, 4], F32); load_col(cg[:], conv_ln_g[0, :])
        cb = alloc(pm, "cb", [128, 4], F32); load_col(cb[:], conv_ln_b[0, :])
        wdw_r = alloc(pm, "wdw_r", [31, 512], F32); S.dma("sp", wdw_r[:], w_dw)
        wdw = alloc(pm, "wdw", [128, 4, 32], F32)
        for ch in range(4):
            S.tr(ps[0][:, ch * 32:ch * 32 + 31], wdw_r[0:31, ch * 128:(ch + 1) * 128], ident_f[0:31, 0:31])
        for ch in range(4):
            S.cp("act", wdw[:, ch, 0:31], ps[0][:, ch * 32:ch * 32 + 31])
        kT_sb = alloc(pm, "kT_sb", [128, 4, 256], BF16)
        v_sb = alloc(pm, "v_sb", [128, 2, 512], BF16)
        with ExitStack() as pmem:
            wmk = alloc(pmem, "wmk", [128, 8, 512], BF16); S.dma("pool", wmk[:], w_mem_k.rearrange("(c p) n -> p c n", p=128))
            wmv = alloc(pmem, "wmv", [128, 8, 512], BF16); S.dma("pool", wmv[:], w_mem_v.rearrange("(c p) n -> p c n", p=128))
            mem_b = alloc(pmem, "mem_b", [128, 2, D], BF16); S.dma("pool", mem_b[:], memb.rearrange("(m p) n -> p m n", p=128))
            memT = alloc(pmem, "memT", [128, 8, 256], BF16)
            for mt in range(2):
                for kc in range(8):
                    S.tr(psT[:, kc * 128:(kc + 1) * 128], mem_b[:, mt, kc * 128:(kc + 1) * 128], ident_b[:])
                S.cp("act", memT[:, :, mt * 128:(mt + 1) * 128], psT[:].rearrange("p (a b) -> p a b", a=8))
            for hh in range(4):
                o = ps[1][:, 0:256]
                for kc in range(8):
                    S.mm(o, wmk[:, kc, hh * 128:(hh + 1) * 128], memT[:, kc, :], start=(kc == 0), stop=(kc == 7))
                S.cp("act", kT_sb[:, hh, :], o)
            for mt in range(2):
                o = ps[2][:]
                for kc in range(8):
                    S.mm(o, memT[:, kc, mt * 128:(mt + 1) * 128], wmv[:, kc, :], start=(kc == 0), stop=(kc == 7))
                S.cp("act", v_sb[:, mt, :], o)
        S.barrier()

        hTg = alloc(pm, "hTg", [128, 8, TG], BF16)
        hTh = alloc(pm, "hTh", [128, 8, 128], BF16)
        ub = alloc(pm, "ub", [128, 4, 30 + TG], F32)
        uh = alloc(pm, "uh", [128, 4, 128], F32)
        sg = alloc(pm, "sg", [128, TG], F32)
        vv = alloc(pm, "vv", [128, 4, TG], F32)
        mean_sb = alloc(pm, "mean_sb", [128, TG], F32)
        rstd_sb = alloc(pm, "rstd_sb", [128, TG], F32)
        xc = alloc(pm, "xc", [128, TG], F32)
        s_b = alloc(pm, "s_b", [128, 4, TG], BF16)
        gt = alloc(pm, "gt", [128, TG], F32)
        tmpm = alloc(pm, "tmpm", [128, TG], F32)
        mixed = alloc(pm, "mixed", [128, 8, TG], F32)
        yag = alloc(pm, "yag", [128, 8, TG], BF16)
        mixed_b = yag
        qc_b = alloc(pm, "qc_b", [128, TG], BF16)
        PTc = alloc(pm, "PTc", [128, 2, TG], BF16)
        rdc = alloc(pm, "rdc", [128, TG], F32)
        oc = alloc(pm, "oc", [128, 4, TG], BF16)
        hres = alloc(pm, "hres", [128, D], F32)
        pre = alloc(pm, "pre", [128, D], F32)
        lnt = alloc(pm, "lnt", [128, D], F32)
        h1t = alloc(pm, "h1t", [128, D], F32)
        h1b = alloc(pm, "h1b", [128, D], BF16)
        h1Tt = alloc(pm, "h1Tt", [128, 8, 128], BF16)

        def glu(hsrc, n, dst):
            for ch in range(4):
                pa_, pg_ = ps[0][:, 0:n], ps[1][:, 0:n]
                for kc in range(8):
                    S.mm(pa_, wglu[:, kc, ch * 128:(ch + 1) * 128], hsrc[:, kc, :], start=(kc == 0), stop=(kc == 7))
                for kc in range(8):
                    S.mm(pg_, wglu[:, kc, 512 + ch * 128:512 + (ch + 1) * 128], hsrc[:, kc, :], start=(kc == 0), stop=(kc == 7))
                S.act(sg[:, 0:n], pg_, AF.Sigmoid, bias=bglu[:, 4 + ch:5 + ch])
                S.stt("dve", dst(ch), pa_, bglu[:, ch:ch + 1], sg[:, 0:n], ALU.add, ALU.mult)

        def gate_chunk(k, dc):
            pg_ = ps[2 + dc % 2][:, 0:TG]
            for kc in range(8):
                S.mm(pg_, wgate[:, kc, k * 1024 + dc * 128:k * 1024 + (dc + 1) * 128], hTg[:, kc, :],
                     start=(kc == 0), stop=(kc == 7))
            S.act(gt[:], pg_, AF.Sigmoid, bias=bgate[:, k * 8 + dc:k * 8 + dc + 1])

        ntg = lim.get("ntg", TQ // TG)
        for tg in range(ntg):
            S.dma("sp", hTg[:], scr_hT[:, :, 128 + tg * TG:128 + (tg + 1) * TG])
            S.dma("sp", yag[:], scr_ya[:, :, tg * TG:(tg + 1) * TG])
            if tg == 0:
                S.dma("sp", hTh[:], scr_hT[:, :, 0:128])
                glu(hTh, 128, lambda ch: uh[:, ch, :])
                for ch in range(4):
                    S.ts("dve", ub[:, ch, 0:30], uh[:, ch, 98:128], cv[:, 3:4], None, ALU.mult)
            else:
                for ch in range(4):
                    S.cp("dve", xc[:, 0:30], ub[:, ch, TG:TG + 30])
                    S.cp("dve", ub[:, ch, 0:30], xc[:, 0:30])
            glu(hTg, TG, lambda ch: ub[:, ch, 30:30 + TG])
            for k in range(31):
                for ch in range(4):
                    if k == 0:
                        S.ts("dve", vv[:, ch, :], ub[:, ch, 0:TG], wdw[:, ch, 0:1], bdw[:, ch:ch + 1], ALU.mult, ALU.add)
                    else:
                        S.stt("dve", vv[:, ch, :], ub[:, ch, k:k + TG], wdw[:, ch, k:k + 1], vv[:, ch, :], ALU.mult, ALU.add)
            for ch in range(4):
                S.mm(ps[2][:, 0:TG], onesf[:], vv[:, ch, :], start=(ch == 0), stop=(ch == 3))
            for ch in range(4):
                S.act(xc[:], vv[:, ch, :], AF.Square)
                S.mm(ps[3][:, 0:TG], onesf[:], xc[:], start=(ch == 0), stop=(ch == 3))
            S.cp("act", mean_sb[:], ps[2][:, 0:TG])
            S.tt("dve", xc[:], mean_sb[:], mean_sb[:], ALU.mult)
            S.tt("dve", rstd_sb[:], ps[3][:, 0:TG], xc[:], ALU.subtract)
            S.act(rstd_sb[:], rstd_sb[:], AF.Sqrt, bias=eps_t[:, 0:1])
            S.recip(rstd_sb[:], rstd_sb[:])
            for ch in range(4):
                S.tt("dve", xc[:], vv[:, ch, :], mean_sb[:], ALU.subtract)
                S.tt("dve", xc[:], xc[:], rstd_sb[:], ALU.mult)
                S.act(s_b[:, ch, :], xc[:], AF.Silu, scale=cg[:, ch:ch + 1], bias=cb[:, ch:ch + 1])
            for dc in range(8):
                gate_chunk(0, dc)
                S.tt("dve", mixed[:, dc, :], yag[:, dc, :], gt[:], ALU.mult)
            for dc in range(8):
                pb_ = ps[dc % 2][:, 0:TG]
                for ch in range(4):
                    S.mm(pb_, wpw2[:, ch, dc * 128:(dc + 1) * 128], s_b[:, ch, :], start=(ch == 0), stop=(ch == 3))
                gate_chunk(1, dc)
                S.stt("dve", tmpm[:], pb_, bpw2[:, dc:dc + 1], gt[:], ALU.add, ALU.mult)
                S.tt("dve", mixed[:, dc, :], mixed[:, dc, :], tmpm[:], ALU.add)
            for hh in range(4):
                pq_ = ps[4][:, 0:TG]
                for kc in range(8):
                    S.mm(pq_, wqc[:, kc, hh * 128:(hh + 1) * 128], hTg[:, kc, :], start=(kc == 0), stop=(kc == 7))
                S.act(qc_b[:], pq_, AF.Identity, bias=bqc[:, hh:hh + 1])
                for mt in range(2):
                    S.mm(ps[5][:, 0:TG], kT_sb[:, hh, mt * 128:(mt + 1) * 128], qc_b[:], start=True, stop=True)
                    S.act(PTc[:, mt, :], ps[5][:, 0:TG], AF.Exp, scale=C_SCALE)
                for mt in range(2):
                    S.mm(ps[6][:, 0:TG], v_sb[:, mt, hh * 128:(hh + 1) * 128], PTc[:, mt, :], start=(mt == 0), stop=(mt == 1))
                for mt in range(2):
                    S.mm(ps[5][:, 0:TG], ones_b[:, 0:128], PTc[:, mt, :], start=(mt == 0), stop=(mt == 1))
                S.recip(rdc[:], ps[5][:, 0:TG])
                S.tt("dve", oc[:, hh, :], ps[6][:, 0:TG], rdc[:], ALU.mult)
            for dc in range(8):
                pc_ = ps[dc % 2][:, 0:TG]
                for hh in range(4):
                    S.mm(pc_, woc[:, hh, dc * 128:(dc + 1) * 128], oc[:, hh, :], start=(hh == 0), stop=(hh == 3))
                gate_chunk(2, dc)
                S.tt("dve", tmpm[:], pc_, gt[:], ALU.mult)
                S.tt("dve", mixed[:, dc, :], mixed[:, dc, :], tmpm[:], ALU.add)
            for dc in range(8):
                S.cp("act", mixed_b[:, dc, :], mixed[:, dc, :])
            for t4 in range(TG // 128):
                ti = tg * (TG // 128) + t4
                S.dma("sp", hres[:], scr_h[ti * 128:(ti + 1) * 128, :])
                for dh in range(2):
                    po_ = ps[dh][:]
                    for dc in range(8):
                        S.mm(po_, mixed_b[:, dc, t4 * 128:(t4 + 1) * 128], wout[:, dc, dh * 512:(dh + 1) * 512],
                             start=(dc == 0), stop=False)
                    S.mm(po_, ones_b[0:1, 0:128], bout[0:1, dh * 512:(dh + 1) * 512], start=False, stop=True)
                    S.stt("dve", pre[:, dh * 512:(dh + 1) * 512], hres[:, dh * 512:(dh + 1) * 512], DN_ALPHA, po_,
                          ALU.mult, ALU.add)
                ln_tok(pre[:], g1_rep[:], b1_rep[:], h1t[:], D, lnt[:])
                S.dma("sp", scr_h1[ti * 128:(ti + 1) * 128, :], h1t[:])
                S.cp("act", h1b[:], h1t[:])
                for kc in range(8):
                    S.tr(psT[:, kc * 128:(kc + 1) * 128], h1b[:, kc * 128:(kc + 1) * 128], ident_b[:])
                S.cp("act", h1Tt[:].rearrange("p a b -> p (a b)"), psT[:])
                S.dma("sp", scr_h1T[:, :, ti * 128:(ti + 1) * 128], h1Tt[:])
        if "mix" in dbg:
            dm = dbg_tensor("h1", [TQ, D], F32)
            S.dma("sp", dm[0:ntg * TG, :], scr_h1[0:ntg * TG, :])
    S.barrier()
    if stage <= 3:
        S.finish()
        return nc, dbg_out, S

    with ExitStack() as pe_:
        g2_rep = alloc(pe_, "g2_rep", [128, D], F32); S.dma("sp", g2_rep[:], ln2_g.partition_broadcast(128))
        b2_rep = alloc(pe_, "b2_rep", [128, D], F32); S.dma("sp", b2_rep[:], ln2_b.partition_broadcast(128))
        h1T = alloc(pe_, "h1T", [128, 8, TQ], BF16)
        S.dma("sp", h1T[:], scr_h1T)
        gates = alloc(pe_, "gates", [128, NT, 32], F32)
        accm = alloc(pe_, "accm", [128, NT, D], F32)
        m8 = alloc(pe_, "m8", [128, 8], F32)
        lg = alloc(pe_, "lg", [128, 32], F32)
        ex = alloc(pe_, "ex", [128, 32], F32)
        sm2 = alloc(pe_, "sm2", [128, 4], F32)
        gT = alloc(pe_, "gT", [32, TQ], BF16)
        gb = alloc(pe_, "gb", [128, 32], BF16)
        pex = ExitStack()
        actT = alloc(pex, "actT", [128, 8, TQ], BF16)
        NRING = 5
        ring = [alloc(pex, "ring%d" % i, [128, 8, 512], BF16) for i in range(NRING)]
        b1r = [alloc(pex, "b1r%d" % i, [1, 2048], BF16) for i in range(1)]
        t1 = [alloc(pex, "t1_%d" % i, [128, 512], F32) for i in range(1)]
        t2 = [alloc(pex, "t2_%d" % i, [128, 512], F32) for i in range(1)]
        t3 = [alloc(pex, "t3_%d" % i, [128, 512], F32) for i in range(1)]
        with ExitStack() as prt:
            wr = alloc(prt, "wr", [128, 8, 32], BF16); S.dma("pool", wr[:], w_router.rearrange("(c p) n -> p c n", p=128))
            br = alloc(prt, "br", [1, 32], BF16); S.dma("pool", br[:], b_router)
            b2_sb = alloc(prt, "b2_sb", [32, D], BF16); S.dma("pool", b2_sb[:], b2)
            for ti in range(NT):
                o = ps[0][:, 0:32]
                for kc in range(8):
                    S.mm(o, h1T[:, kc, ti * 128:(ti + 1) * 128], wr[:, kc, :], start=(kc == 0), stop=False)
                S.mm(o, ones_b[0:1, 0:128], br[0:1, :], start=False, stop=True)
                S.cp("act", lg[:], o)
                S.max8(m8[:], lg[:])
                S.ts("dve", ex[:], lg[:], m8[:, 0:1], None, ALU.subtract)
                S.act(ex[:], ex[:], AF.Exp)
                S.stt("dve", ex[:], lg[:], m8[:, 3:4], ex[:], ALU.is_ge, ALU.mult)
                S.red(sm2[:, 0:1], ex[:], ALU.add)
                S.recip(sm2[:, 1:2], sm2[:, 0:1])
                S.ts("dve", gates[:, ti, :], ex[:], sm2[:, 1:2], None, ALU.mult)
                S.cp("act", gb[:], gates[:, ti, :])
                S.tr(psT[0:32, 0:128], gb[:], ident_b[:])
                S.cp("act", gT[:, ti * 128:(ti + 1) * 128], psT[0:32, 0:128])
                for dh in range(2):
                    S.mm(ps[1 + dh][:], gT[:, ti * 128:(ti + 1) * 128], b2_sb[:, dh * 512:(dh + 1) * 512], start=True, stop=True)
                    S.cp("act", accm[:, ti, dh * 512:(dh + 1) * 512], ps[1 + dh][:])
        S.barrier()
        if "moe" in dbg:
            dg_ = dbg_tensor("gates", [128, NT * 32], F32)
            S.dma("sp", dg_, gates[:].rearrange("p a b -> p (a b)"))
        w1k = w1.rearrange("e (c p) n -> e p c n", p=128)
        w2k = w2.rearrange("e (c p) n -> e p c n", p=128)
        nexp = lim.get("nexp", 32)
        nr = 0
        nel = 0
        for e in range(nexp):
            brow = b1r[0]
            S.dma("pool", brow[:], b1[e:e + 1, :])
            for c in range(4):
                wc = ring[nr % NRING]
                nr += 1
                S.dma("pool", wc[:, :, 0:256], w1k[e, :, :, 256 * c:256 * c + 256])
                S.dma("pool", wc[:, :, 256:512], w1k[e, :, :, 1024 + 256 * c:1024 + 256 * c + 256])
                for tg in range(4):
                    rhs_t = slice(tg * 512, (tg + 1) * 512)
                    for fc in range(2):
                        pg_, pu_ = ps[2 * fc][:], ps[2 * fc + 1][:]
                        for (po_, off, boff) in ((pg_, fc * 128, 256 * c + fc * 128),
                                                 (pu_, 256 + fc * 128, 1024 + 256 * c + fc * 128)):
                            for kc in range(8):
                                S.mm(po_, wc[:, kc, off:off + 128], h1T[:, kc, rhs_t], start=(kc == 0), stop=False)
                            S.mm(po_, brow[0:1, boff:boff + 128], ones_b[0:1, 0:512], start=False, stop=True)
                        a1, a2, a3 = t1[0], t2[0], t3[0]
                        nel += 1
                        S.ts("dve", a1[:], pg_, 7.0, None, ALU.min)
                        S.act(a2[:], a1[:], AF.Sigmoid, scale=1.702)
                        S.tt("dve", a1[:], a1[:], a2[:], ALU.mult)
                        S.ts("dve", a3[:], pu_, -7.0, 7.0, ALU.max, ALU.min)
                        S.stt("dve", actT[:, 2 * c + fc, rhs_t], a3[:], 1.0, a1[:], ALU.add, ALU.mult)
            for dh in range(2):
                wc = ring[nr % NRING]
                nr += 1
                S.dma("pool", wc[:], w2k[e, :, :, dh * 512:(dh + 1) * 512])
                for ti in range(NT):
                    py = ps[4 + ti % 3][:]
                    for fc in range(8):
                        S.mm(py, actT[:, fc, ti * 128:(ti + 1) * 128], wc[:, fc, :], start=(fc == 0), stop=(fc == 7))
                    S.stt("dve", accm[:, ti, dh * 512:(dh + 1) * 512], py, gates[:, ti, e:e + 1],
                          accm[:, ti, dh * 512:(dh + 1) * 512], ALU.mult, ALU.add)
        pex.close()
        S.barrier()
        h1r = [alloc(pe_, "h1r%d" % i, [128, D], F32) for i in range(2)]
        lnt2 = alloc(pe_, "lnt2", [128, D], F32)
        yo = [alloc(pe_, "yo%d" % i, [128, D], F32) for i in range(2)]
        for ti in range(NT):
            hr = h1r[ti % 2]
            S.dma("sp", hr[:], scr_h1[ti * 128:(ti + 1) * 128, :])
            S.stt("dve", hr[:], hr[:], DN_ALPHA, accm[:, ti, :], ALU.mult, ALU.add)
            ln_tok(hr[:], g2_rep[:], b2_rep[:], yo[ti % 2][:], D, lnt2[:])
            S.dma("sp", y_out[ti * 128:(ti + 1) * 128, :], yo[ti % 2][:])
    S.finish()
    return nc, dbg_out, S


def _prep_inputs(inputs, stage=99):
    x = np.asarray(inputs["x"], np.float32)
    mem = np.asarray(inputs["mem"], np.float32)
    common = {}
    sq = {"ln0_g": (1, D), "ln0_b": (1, D), "w_in": (D, D_IN), "b_in": (1, D_IN), "kv_norm_g": (1, 128)}
    if stage >= 2:
        sq.update({"w_uk": (8, 128, 64), "w_uv": (8, 128, 64), "w_o_a": (512, D)})
    if stage >= 3:
        sq.update({"w_dw": (31, 512), "b_dw": (1, 512), "conv_ln_g": (1, 512), "conv_ln_b": (1, 512),
                   "w_pw2": (512, D), "b_pw2": (1, D), "w_mem_k": (D, 512), "w_mem_v": (D, 512),
                   "w_o_c": (512, D), "w_out": (D, D), "b_out": (1, D), "ln1_g": (1, D), "ln1_b": (1, D)})
    if stage >= 4:
        sq.update({"w_router": (D, 32), "b_router": (1, 32), "w1": (32, D, 2048), "b1": (32, 2048),
                   "w2": (32, D, D), "b2": (32, D), "ln2_g": (1, D), "ln2_b": (1, D)})
    for k, shp in sq.items():
        common[k] = np.ascontiguousarray(np.asarray(inputs[k], np.float32).reshape(shp))
    for k, v in _consts().items():
        common["c_" + k] = v
    in_maps = []
    for c in range(8):
        b, j = c // 4, c % 4
        m = dict(common)
        m["xk"] = np.ascontiguousarray(np.roll(x[b], -(j + 1) * TQ, axis=0))
        if stage >= 3:
            m["memb"] = np.ascontiguousarray(mem[b])
        cvv = np.zeros((128, 8), np.float32)
        for q in range(3):
            cvv[:, q] = 0.0 if q >= 3 - j else NEG
        cvv[:, 3] = 1.0 if j > 0 else 0.0
        m["cvec"] = cvv
        in_maps.append(m)
    return in_maps


def kernel(**inputs):
    in_maps = _prep_inputs(inputs)
    nc, _, _ = build()
    res = run_bass_kernel_spmd(nc, in_maps, core_ids=list(range(8)))
    out = np.zeros((2, SEQ, D), np.float32)
    for c in range(8):
        b, j = c // 4, c % 4
        out[b, j * TQ:(j + 1) * TQ] = np.asarray(res.results[c]["y"], np.float32)
    return out
```
